# Optimizing a Trainium2 kernel written in Bass

```python
import jax, jax.numpy as jnp
from jax import lax
import numpy as np

D_MODEL = 2048
BATCH = 4
SEQ = 8192
DEPTH = 1
DEC_BATCH = 1
DEC_SEQ = 16384
PAST_LEN = 128

CHUNK = 128
G_GROUPS = 8
G_WIDTH = D_MODEL
G_GDIM = G_WIDTH // G_GROUPS
M_HEADS = 8
M_WIDTH = D_MODEL
M_HDIM = M_WIDTH // M_HEADS
CONV_W = 5
N_GROUPS = 4
EXPERTS_PER_GROUP = 8
N_EXPERTS = N_GROUPS * EXPERTS_PER_GROUP
TOP_K_IN_GROUP = 2
D_EXPERT = 1024
MOE_BLOCK = 128
EPS = 1e-6

OFF_GU = 0
OFF_GV = OFF_GU + G_WIDTH
OFF_Q = OFF_GV + G_WIDTH
OFF_K = OFF_Q + M_WIDTH
OFF_V = OFF_K + M_WIDTH
OFF_O = OFF_V + M_WIDTH
OFF_CG = OFF_O + M_WIDTH
N_CELL_GATES = 4 * M_HEADS
OFF_MERGE = OFF_CG + N_CELL_GATES
D_IN = OFF_MERGE + 2 * D_MODEL

kernel_name = "hybrid_gmlp_mlstm_hmoe_encoder"


def rmsnorm(x, g):
    xf = x.astype(jnp.float32)
    y = xf * lax.rsqrt(jnp.mean(xf * xf, axis=-1, keepdims=True) + EPS)
    return (y * g.astype(jnp.float32)).astype(x.dtype)


def gmlp_branch(u, v, ln_g, w_s, b_s):
    B, S, _ = u.shape
    vf = v.astype(jnp.float32)
    mu = jnp.mean(vf, axis=-1, keepdims=True)
    var = jnp.mean(jnp.square(vf - mu), axis=-1, keepdims=True)
    vn = ((vf - mu) * lax.rsqrt(var + EPS) * ln_g.astype(jnp.float32)).astype(u.dtype)
    vc = vn.reshape(B, S // CHUNK, CHUNK, G_GROUPS, G_GDIM)
    s = jnp.einsum('gts,bcsgd->bctgd', w_s, vc) + b_s.T[:, :, None]
    return u * s.reshape(B, S, G_WIDTH)


def centred_dwconv(x, w):
    C = x.shape[-1]
    return lax.conv_general_dilated(
        x, w.astype(x.dtype)[:, None, :], window_strides=(1,),
        padding=[(CONV_W // 2, CONV_W // 2)],
        dimension_numbers=('NWC', 'WIO', 'NWC'), feature_group_count=C)


def mlstm_scan(q, k, v, log_i, log_f):
    B, H, S, D = q.shape
    nc = S // CHUNK
    to_chunks = lambda a: jnp.moveaxis(a.reshape(B, H, nc, CHUNK, *a.shape[3:]), 2, 0)
    tril = jnp.tril(jnp.ones((CHUNK, CHUNK), dtype=bool))

    def step(carry, inp):
        C, n, m = carry
        qc, kc, vc, lic, lfc = inp
        F = jnp.cumsum(lfc, axis=-1)
        Dm = F[..., :, None] - F[..., None, :] + lic[..., None, :]
        Dm = jnp.where(tril, Dm, -jnp.inf)
        inter = F + m[..., None]
        m_t = jnp.maximum(inter, jnp.max(Dm, axis=-1))
        w_inter = jnp.exp(inter - m_t)
        Sm = jnp.einsum('bhtd,bhsd->bhts', qc, kc) * jnp.exp(Dm - m_t[..., None])
        num = (w_inter[..., None] * jnp.einsum('bhtd,bhde->bhte', qc, C)
               + jnp.einsum('bhts,bhse->bhte', Sm, vc))
        den = w_inter * jnp.einsum('bhtd,bhd->bht', qc, n) + jnp.sum(Sm, axis=-1)
        h = num / jnp.maximum(jnp.abs(den), jnp.exp(-m_t))[..., None]
        F_L = F[..., -1]
        g_s = F_L[..., None] - F + lic
        m_new = jnp.maximum(F_L + m, jnp.max(g_s, axis=-1))
        a = jnp.exp(F_L + m - m_new)
        kw = kc * jnp.exp(g_s - m_new[..., None])[..., None]
        C_new = a[..., None, None] * C + jnp.einsum('bhsd,bhse->bhde', kw, vc)
        n_new = a[..., None] * n + jnp.sum(kw, axis=2)
        return (C_new, n_new, m_new), h

    init = (jnp.zeros((B, H, D, D), jnp.float32), jnp.zeros((B, H, D), jnp.float32),
            jnp.zeros((B, H), jnp.float32))
    _, hs = lax.scan(step, init, (to_chunks(q), to_chunks(k), to_chunks(v),
                                  to_chunks(log_i), to_chunks(log_f)))
    return jnp.moveaxis(hs, 0, 2).reshape(B, H, S, D)


def mlstm_branch(q_raw, k_raw, v_raw, o_raw, cg_raw, conv_w, b_cg, head_g):
    B, S, _ = q_raw.shape
    qk = jax.nn.silu(centred_dwconv(jnp.concatenate([q_raw, k_raw], axis=-1), conv_w))
    heads = lambda a: a.astype(jnp.float32).reshape(B, S, M_HEADS, M_HDIM).transpose(0, 2, 1, 3)
    q = heads(qk[..., :M_WIDTH])
    k = heads(qk[..., M_WIDTH:]) * (M_HDIM ** -0.5)
    v = heads(v_raw)
    g = (cg_raw.astype(jnp.float32) + b_cg.astype(jnp.float32)).reshape(B, S, 4, M_HEADS)
    g = g.transpose(2, 0, 3, 1)
    li_f, lf_f = g[0], jax.nn.log_sigmoid(g[1])
    li_b, lf_b = g[2], jax.nn.log_sigmoid(g[3])
    h_f = mlstm_scan(q, k, v, li_f, lf_f)
    flip = lambda a: jnp.flip(a, axis=2)
    h_b = flip(mlstm_scan(flip(q), flip(k), flip(v), flip(li_b), flip(lf_b)))
    h = h_f + h_b
    h = h * lax.rsqrt(jnp.mean(h * h, axis=-1, keepdims=True) + EPS) * head_g.astype(jnp.float32)[None, :, None, :]
    h = h.transpose(0, 2, 1, 3).reshape(B, S, M_WIDTH).astype(q_raw.dtype)
    return h * jax.nn.sigmoid(o_raw)


def token_mixer(xn, w_in, b_cg, gmlp_ln_g, gmlp_w_s, gmlp_b_s, conv_w, head_g, w_out):
    z = xn @ w_in
    gu = jax.nn.gelu(z[..., OFF_GU:OFF_GV])
    gv = jax.nn.gelu(z[..., OFF_GV:OFF_Q])
    a = gmlp_branch(gu, gv, gmlp_ln_g, gmlp_w_s, gmlp_b_s)
    b = mlstm_branch(z[..., OFF_Q:OFF_K], z[..., OFF_K:OFF_V], z[..., OFF_V:OFF_O],
                     z[..., OFF_O:OFF_CG], z[..., OFF_CG:OFF_MERGE], conv_w, b_cg, head_g)
    gate_a = jax.nn.sigmoid(z[..., OFF_MERGE:OFF_MERGE + D_MODEL])
    gate_b = jax.nn.sigmoid(z[..., OFF_MERGE + D_MODEL:])
    return (gate_a * a + gate_b * b) @ w_out


def hier_moe(x, w_rg, b_rg, w_re, b_re, w_gate, w_up, w_down):
    B, S, Dm = x.shape
    T = B * S
    xt = x.reshape(T, Dm)
    g_logits = (xt @ w_rg).astype(jnp.float32) + b_rg.astype(jnp.float32)
    g_prob = jax.nn.softmax(g_logits, axis=-1)
    _, g_sel = lax.top_k(g_logits, 1)
    p_g = jnp.take_along_axis(g_prob, g_sel, axis=1)
    e_logits = ((xt @ w_re).astype(jnp.float32) + b_re.astype(jnp.float32)).reshape(T, N_GROUPS, EXPERTS_PER_GROUP)
    e_in = jnp.take_along_axis(e_logits, g_sel[:, :, None], axis=1)[:, 0]
    top_v, top_i = lax.top_k(e_in, TOP_K_IN_GROUP)
    weights = p_g * jax.nn.softmax(top_v, axis=-1)
    expert_id = g_sel * EXPERTS_PER_GROUP + top_i

    N = T * TOP_K_IN_GROUP
    flat_e = expert_id.reshape(N)
    flat_tok = jnp.arange(N, dtype=jnp.int32) // TOP_K_IN_GROUP
    flat_w = weights.reshape(N)
    order = jnp.argsort(flat_e)
    se, stok, sw = flat_e[order], flat_tok[order], flat_w[order]
    counts = jnp.bincount(flat_e, length=N_EXPERTS)
    padded = ((counts + MOE_BLOCK - 1) // MOE_BLOCK) * MOE_BLOCK
    pad_end = jnp.cumsum(padded)
    pad_start = pad_end - padded
    raw_start = jnp.cumsum(counts) - counts
    dest = pad_start[se] + (jnp.arange(N, dtype=jnp.int32) - raw_start[se])
    n_blocks = (N + N_EXPERTS * (MOE_BLOCK - 1) + MOE_BLOCK - 1) // MOE_BLOCK
    buf = jnp.zeros((n_blocks * MOE_BLOCK, Dm), x.dtype).at[dest].set(xt[stok])
    block_e = jnp.searchsorted(pad_end, jnp.arange(n_blocks, dtype=jnp.int32) * MOE_BLOCK, side='right')
    block_e = jnp.minimum(block_e, N_EXPERTS - 1)

    def expert_block(args):
        xb, e = args
        h = jax.nn.silu(xb @ w_gate[e]) * (xb @ w_up[e])
        return h @ w_down[e]

    out = lax.map(expert_block, (buf.reshape(n_blocks, MOE_BLOCK, Dm), block_e))
    out = out.reshape(n_blocks * MOE_BLOCK, Dm)
    y_assign = out[dest] * sw[:, None].astype(x.dtype)
    y = jax.ops.segment_sum(y_assign, stok, num_segments=T)
    return y.reshape(B, S, Dm)


def trunk(x, norm_mix_g, norm_ffn_g, norm_final_g, w_in, b_cell_gates, gmlp_ln_g, gmlp_w_s,
          gmlp_b_s, mlstm_conv_w, mlstm_head_g, w_out, w_router_group, b_router_group,
          w_router_expert, b_router_expert, w_exp_gate, w_exp_up, w_exp_down):
    for l in range(DEPTH):
        h = rmsnorm(x, norm_mix_g[l])
        x = x + token_mixer(h, w_in[l], b_cell_gates[l], gmlp_ln_g[l], gmlp_w_s[l], gmlp_b_s[l],
                            mlstm_conv_w[l], mlstm_head_g[l], w_out[l])
        h = rmsnorm(x, norm_ffn_g[l])
        x = x + hier_moe(h, w_router_group[l], b_router_group[l], w_router_expert[l],
                         b_router_expert[l], w_exp_gate[l], w_exp_up[l], w_exp_down[l])
    return rmsnorm(x, norm_final_g)


def setup_inputs(seed: int = 0) -> dict:
    key = jax.random.key(seed)
    ks = jax.random.split(key, 24)
    f32 = jnp.float32
    nrm = lambda k, shape, s: jax.random.normal(k, shape, f32) * s
    i_bias = lambda k: nrm(k, (DEPTH, M_HEADS), 0.1)
    f_bias = lambda k: 3.0 + 3.0 * jax.random.uniform(k, (DEPTH, M_HEADS), f32)
    b_cell_gates = jnp.concatenate([i_bias(ks[5]), f_bias(ks[6]), i_bias(ks[7]), f_bias(ks[8])], axis=-1)
    return {
        "x_prompt": nrm(ks[0], (BATCH, SEQ, D_MODEL), 1.0),
        "x_sample": nrm(ks[1], (DEC_BATCH, DEC_SEQ, D_MODEL), 1.0),
        "norm_mix_g": 1.0 + nrm(ks[2], (DEPTH, D_MODEL), 0.01),
        "norm_ffn_g": 1.0 + nrm(ks[3], (DEPTH, D_MODEL), 0.01),
        "norm_final_g": 1.0 + nrm(ks[4], (D_MODEL,), 0.01),
        "w_in": nrm(ks[9], (DEPTH, D_MODEL, D_IN), D_MODEL ** -0.5),
        "b_cell_gates": b_cell_gates,
        "gmlp_ln_g": 1.0 + nrm(ks[10], (DEPTH, G_WIDTH), 0.01),
        "gmlp_w_s": nrm(ks[11], (DEPTH, G_GROUPS, CHUNK, CHUNK), CHUNK ** -0.5),
        "gmlp_b_s": 1.0 + nrm(ks[12], (DEPTH, G_GROUPS, CHUNK), 0.1),
        "mlstm_conv_w": nrm(ks[13], (DEPTH, CONV_W, 2 * M_WIDTH), CONV_W ** -0.5),
        "mlstm_head_g": 1.0 + nrm(ks[14], (DEPTH, M_HEADS, M_HDIM), 0.01),
        "w_out": nrm(ks[15], (DEPTH, D_MODEL, D_MODEL), D_MODEL ** -0.5),
        "w_router_group": nrm(ks[16], (DEPTH, D_MODEL, N_GROUPS), D_MODEL ** -0.5),
        "b_router_group": nrm(ks[17], (DEPTH, N_GROUPS), 0.01),
        "w_router_expert": nrm(ks[18], (DEPTH, D_MODEL, N_EXPERTS), D_MODEL ** -0.5),
        "b_router_expert": nrm(ks[19], (DEPTH, N_EXPERTS), 0.01),
        "w_exp_gate": nrm(ks[20], (DEPTH, N_EXPERTS, D_MODEL, D_EXPERT), D_MODEL ** -0.5),
        "w_exp_up": nrm(ks[21], (DEPTH, N_EXPERTS, D_MODEL, D_EXPERT), D_MODEL ** -0.5),
        "w_exp_down": nrm(ks[22], (DEPTH, N_EXPERTS, D_EXPERT, D_MODEL), D_EXPERT ** -0.5),
    }


def reference(x_prompt, x_sample, norm_mix_g, norm_ffn_g, norm_final_g, w_in, b_cell_gates,
              gmlp_ln_g, gmlp_w_s, gmlp_b_s, mlstm_conv_w, mlstm_head_g, w_out, w_router_group,
              b_router_group, w_router_expert, b_router_expert, w_exp_gate, w_exp_up, w_exp_down):
    y_prompt = trunk(x_prompt, norm_mix_g, norm_ffn_g, norm_final_g, w_in, b_cell_gates, gmlp_ln_g,
                     gmlp_w_s, gmlp_b_s, mlstm_conv_w, mlstm_head_g, w_out, w_router_group,
                     b_router_group, w_router_expert, b_router_expert, w_exp_gate, w_exp_up, w_exp_down)
    y_sample = trunk(x_sample, norm_mix_g, norm_ffn_g, norm_final_g, w_in, b_cell_gates, gmlp_ln_g,
                     gmlp_w_s, gmlp_b_s, mlstm_conv_w, mlstm_head_g, w_out, w_router_group,
                     b_router_group, w_router_expert, b_router_expert, w_exp_gate, w_exp_up, w_exp_down)
    return (y_prompt, y_sample)
```

```python
import numpy as np
import concourse.bass as bass
import concourse.mybir as mybir
from concourse.bass_utils import run_bass_kernel_spmd

F32 = mybir.dt.float32
BF16 = mybir.dt.bfloat16
I32 = mybir.dt.int32
AF = mybir.ActivationFunctionType
ALU = mybir.AluOpType
AX = mybir.AxisListType

D = 2048
DIN = 16416
NEXP = 32
DEXP = 1024
EPS = 1e-6
NEG = -30000.0
SEM_CHUNK = 20000


class Buf:
    __slots__ = ("name", "lw", "lr")

    def __init__(self, name):
        self.name = name
        self.lw = {}
        self.lr = {}


class Sched:
    def __init__(self, nc, n_dma_slots=8):
        self.nc = nc
        self.eng = {"pe": nc.tensor, "act": nc.scalar, "dve": nc.vector, "pool": nc.gpsimd, "sp": nc.sync}
        self.ops = []
        self.n_dma_slots = n_dma_slots
        self.dma_rr = {"sp": 0, "act": 0, "pool": 0}
        self.base = 0
        self.floor = 0
        self.seen = {e: {} for e in self.eng}
        self.last_on_slot = {}
        self.cnt = {}
        self.sems = {}
        self.n_waits = 0
        self.n_ops = 0

    def op(self, eng, fn, reads=(), writes=()):
        self.ops.append((eng, fn, tuple(reads), tuple(writes), None))

    def dma(self, q, fn, reads=(), writes=(), group=None, nslots=None):
        key = q if group is None else (q, group)
        n = self.n_dma_slots if nslots is None else nslots
        i = self.dma_rr.get(key, 0)
        self.dma_rr[key] = (i + 1) % n
        slot = i if group is None else "%s%d" % (group, i)
        self.ops.append((q, fn, tuple(reads), tuple(writes), slot))

    def _sem_for(self, t, k):
        step = 1 if not isinstance(t, tuple) else 16
        ch = SEM_CHUNK if step == 1 else SEM_CHUNK // 16
        c = (k - 1) // ch
        key = (t, c)
        if key not in self.sems:
            nm = ("s_%s_%d" % (t, c)) if not isinstance(t, tuple) else ("d_%s%s_%d" % (t[1], t[2], c))
            self.sems[key] = self.nc.semaphore(nm).__enter__()
        return self.sems[key], ((k - 1) % ch + 1) * step

    def flush(self):
        ops = self.ops
        n = len(ops)
        base = self.base
        track_of = [None] * n
        deps = [None] * n
        floor = self.floor
        for li, (e, fn, reads, writes, slot) in enumerate(ops):
            i = base + li
            tr = e if slot is None else ("dma", e, slot)
            track_of[li] = tr
            d = {}
            same = (slot is None)

            def need(t, j):
                if j is None or j < floor:
                    return
                if same and t == e and (e == "pe" or e == "sp"):
                    return
                if d.get(t, -1) < j:
                    d[t] = j

            for b in reads:
                for t, j in b.lw.items():
                    need(t, j)
            for b in writes:
                for t, j in b.lw.items():
                    need(t, j)
                for t, j in b.lr.items():
                    if same and t == e:
                        continue
                    need(t, j)
            if slot is not None:
                need(tr, self.last_on_slot.get(tr))
                self.last_on_slot[tr] = i
            sd = self.seen[e]
            fd = {}
            for t, j in d.items():
                if sd.get(t, -1) < j:
                    sd[t] = j
                    fd[t] = j
            deps[li] = fd
            for b in reads:
                b.lr[tr] = i
            for b in writes:
                b.lw[tr] = i
        milestone = [False] * n
        for li in range(n):
            for t, j in deps[li].items():
                milestone[j - base] = True
            if ops[li][4] is not None:
                milestone[li] = True
        last_of_track = {}
        for li in range(n):
            last_of_track[track_of[li]] = li
        for t, lj in last_of_track.items():
            milestone[lj] = True
        ev = [None] * n
        for li in range(n):
            if milestone[li]:
                t = track_of[li]
                self.cnt[t] = self.cnt.get(t, 0) + 1
                ev[li] = self.cnt[t]
        for li, (e, fn, reads, writes, slot) in enumerate(ops):
            eng = self.eng[e]
            for t, j in deps[li].items():
                s, v = self._sem_for(t, ev[j - base])
                eng.wait_ge(s, v)
                self.n_waits += 1
            ins = fn()
            if milestone[li]:
                s, v = self._sem_for(track_of[li], ev[li])
                ins.then_inc(s, 16 if slot is not None else 1)
        for t, lj in last_of_track.items():
            s, v = self._sem_for(t, ev[lj])
            for en, eng in self.eng.items():
                if en == t:
                    continue
                eng.wait_ge(s, v)
        self.n_ops += n
        self.base = base + n
        self.floor = self.base
        self.ops = []


class Pools:
    def __init__(self, nc):
        self.nc = nc
        self.stack = []

    def sb(self, name, shape, dt):
        cm = self.nc.sbuf_tensor("sb_" + name, shape, dt)
        h = cm.__enter__()
        self.stack.append(cm)
        return h

    def ps(self, name, shape, dt):
        cm = self.nc.psum_tensor("ps_" + name, shape, dt)
        h = cm.__enter__()
        self.stack.append(cm)
        return h

    def mark(self):
        return len(self.stack)

    def release(self, mark):
        while len(self.stack) > mark:
            self.stack.pop().__exit__(None, None, None)


def build(NOWN, NCTX, CAP, debug=False):
    assert NOWN % 4 == 0 and NCTX % 4 == 0 and CAP % 128 == 0
    NCH = NOWN + NCTX
    NT = NCH * 128
    NTO = NOWN * 128
    NTILE = NCH // 4
    NTILE_OWN = NOWN // 4
    BLK = 512
    NBLK = (NTO * 2) // BLK + NEXP
    if NBLK % 2:
        NBLK += 1
    NL = NBLK * BLK
    TRASH = NL
    nc = bass.Bass("TRN2", target_bir_lowering=False)
    S = Sched(nc)
    P = Pools(nc)
    skind = "ExternalOutput" if debug else "Internal"

    def din(name, shape, dt=F32):
        return nc.dram_tensor(name, list(shape), dt, kind="ExternalInput").ap()

    def dscr(name, shape, dt):
        return nc.dram_tensor(name, list(shape), dt, kind=skind).ap()

    x_d = din("x", [NT, D])
    valid_d = din("valid", [128, NOWN])
    keep_d = din("keep", [128, 1])
    w_in_d = din("w_in", [D, DIN])
    g_mix_d = din("g_mix", [1, D])
    g_ffn_d = din("g_ffn", [1, D])
    g_fin_d = din("g_fin", [1, D])
    b_cg_d = din("b_cg", [1, 32])
    ln_g_d = din("ln_g", [1, D])
    wsT_d = din("wsT", [128, 8, 128])
    bsT_d = din("bsT", [128, 8])
    convw_d = din("convw", [128, 32, 5])
    headg_d = din("headg", [1, D])
    w_out_d = din("w_out", [D, D])
    w_r_d = din("w_r", [D, 36])
    b_r_d = din("b_r", [1, 36])
    wg_d = din("w_gate", [NEXP * 4 * 128, 16 * 256])
    wu_d = din("w_up", [NEXP * 4 * 128, 16 * 256])
    wd_d = din("w_down", [NEXP * 4 * 128, 8 * 512])
    cst_d = din("cst", [128, 1024])
    sel_d = din("sel", [8, 8 * 128 + 128 + 8])
    rcst_d = din("rcst", [128, 240])
    padi_d = din("padi", [128, NL // 128 + 1], I32)
    y_d = nc.dram_tensor("y", [NTO, D], F32, kind="ExternalOutput").ap()

    SL_C0 = ([2048 + 512 * i for i in range(4)] + [512 * i for i in range(4)] + [12320 + 512 * i for i in range(4)]
             + [10240 + 512 * i for i in range(4)] + [14368 + 512 * i for i in range(4)] + [8192 + 512 * i for i in range(4)]
             + [12288] + [4096 + 512 * i for i in range(8)])
    SID = {c0: i for i, c0 in enumerate(SL_C0)}
    winb = dscr("winb", [len(SL_C0), 128, 16, 512], BF16)
    QT = dscr("QT", [NCH, 128, 16, 128], BF16)
    KT = dscr("KT", [NCH, 128, 16, 128], BF16)
    Vs = dscr("Vs", [NT, D], BF16)
    A1s = dscr("A1s", [NT, D], BF16)
    G2s = dscr("G2s", [NT, D], BF16)
    GAT = dscr("GAT", [NT, 32], F32)
    HFs = dscr("HFs", [NT, D], F32)
    X1s = dscr("X1s", [NT, D], F32)
    H2s = dscr("H2s", [NTO + 128, D], BF16)
    Ls = dscr("Ls", [NL + 128, 1], I32)
    NLh = NL // 2
    YsA = dscr("YsA", [NLh + 128, D], F32)
    YsB = dscr("YsB", [NLh + 128, D], F32)
    WGB = dscr("WGB", [NEXP * 4 * 128, 16 * 256], BF16)
    WUB = dscr("WUB", [NEXP * 4 * 128, 16 * 256], BF16)
    WDB = dscr("WDB", [NEXP * 4 * 128, 8 * 512], BF16)
    B_wexp = Buf("wexp")
    cast_jobs = []
    for r_ in range(NEXP * 4):
        for (dst_, src_) in ((WGB, wg_d), (WUB, wu_d), (WDB, wd_d)):
            cast_jobs.append((dst_, src_, r_))
    cast_pos = [0]

    def issue_casts(n_):
        for (dst_, src_, r_) in cast_jobs[cast_pos[0]:cast_pos[0] + n_]:
            S.dma("pool", lambda dst_=dst_, src_=src_, r_=r_: nc.gpsimd.dma_start(out=dst_[r_ * 128:(r_ + 1) * 128, :],
                                                                                 in_=src_[r_ * 128:(r_ + 1) * 128, :]),
                  (), [B_wexp], group="cast", nslots=6)
        cast_pos[0] += n_
    n_iter_mlstm = NOWN + NCH
    casts_per_iter = (len(cast_jobs) + n_iter_mlstm - 1) // n_iter_mlstm
    B_winb = [Buf("winb%d" % i) for i in range(64)]
    B_QT = [Buf("QT%d" % j) for j in range(NCH)]
    B_KT = [Buf("KT%d" % j) for j in range(NCH)]
    B_V = [Buf("V%d" % j) for j in range(NCH)]
    B_A1 = [Buf("A1%d" % j) for j in range(NCH)]
    B_G2 = [Buf("G2%d" % j) for j in range(NCH)]
    B_GAT = [Buf("GAT%d" % j) for j in range(NCH)]
    B_HF = [Buf("HF%d" % j) for j in range(NCH)]
    B_X1 = [Buf("X1%d" % j) for j in range(NCH)]
    B_H2 = Buf("H2")
    B_L = Buf("L")
    B_Y = Buf("Y")

    cst = P.sb("cst", [128, 1024], F32)
    B_cst = Buf("cst")
    identf = cst[:, 0:128]
    T_f, MA_f, MAT_f = cst[:, 128:256], cst[:, 256:384], cst[:, 384:512]
    T_b, MA_b, MAT_b = cst[:, 512:640], cst[:, 640:768], cst[:, 768:896]
    trist = cst[:, 896:1024]
    selt = P.sb("selt", [8, 8 * 128 + 128 + 8], F32)
    B_sel = Buf("sel")
    ones8 = selt[:, 1024:1152]
    ident8 = selt[:, 1152:1160]
    identb = P.sb("identb", [128, 128], BF16)
    tristb = P.sb("tristb", [128, 128], BF16)
    onesb = P.sb("onesb", [128, 128], BF16)
    B_cb = Buf("constb")
    epsb = P.sb("epsb", [128, 1], F32)
    validt = P.sb("validt", [128, NOWN], F32)
    keept = P.sb("keept", [128, 1], F32)
    rcst = P.sb("rcst", [128, 240], F32)
    iota32 = rcst[:, 48:80]
    iotaB = rcst[:, 80:208]
    ones32 = rcst[:, 208:240]
    pe_all = P.sb("pe_all", [128, NOWN, 4], F32)
    widx = P.sb("widx", [128, 4, 128], I32)
    sslot = P.sb("sslot", [128, 32], F32)
    B_bexp = Buf("bexp")
    B_ss = Buf("sslot")
    iotap = rcst[:, 40:41]
    onec = rcst[:, 41:42]
    dest_all = P.sb("dest_all", [128, NOWN, 6], I32)
    w_all = P.sb("w_all", [128, NOWN, 2], F32)
    B_dest = [Buf("dest%d" % j) for j in range(NCH)]
    B_misc = Buf("misc")

    def V(fn, r=(), w=()):
        S.op("dve", fn, r, w)

    def A(fn, r=(), w=()):
        S.op("act", fn, r, w)

    def G(fn, r=(), w=()):
        S.op("pool", fn, r, w)

    def T(fn, r=(), w=()):
        S.op("pe", fn, r, w)

    def Dsp(fn, r=(), w=()):
        S.dma("sp", fn, r, w)

    def Dpl(fn, r=(), w=()):
        S.dma("pool", fn, r, w)

    def bcast_row(dram_row, n):
        return dram_row.partition_broadcast(128)

    Dsp(lambda: nc.sync.dma_start(out=cst[:], in_=cst_d), w=[B_cst])
    Dsp(lambda: nc.sync.dma_start(out=selt[:], in_=sel_d), w=[B_sel])
    Dsp(lambda: nc.sync.dma_start(out=validt[:], in_=valid_d), w=[B_misc])
    Dsp(lambda: nc.sync.dma_start(out=keept[:], in_=keep_d), w=[B_misc])
    V(lambda: nc.vector.tensor_copy(out=identb[:], in_=identf), r=[B_cst], w=[B_cb])
    V(lambda: nc.vector.tensor_copy(out=tristb[:], in_=trist), r=[B_cst], w=[B_cb])
    G(lambda: nc.gpsimd.memset(onesb[:], 1.0), w=[B_cb])
    G(lambda: nc.gpsimd.memset(epsb[:], EPS), w=[B_misc])
    Dsp(lambda: nc.sync.dma_start(out=rcst[:], in_=rcst_d), w=[B_misc])
    for sid_, c0_ in enumerate(SL_C0):
        ncl_ = 32 if c0_ == 12288 else 512
        Dpl(lambda sid_=sid_, c0_=c0_, ncl_=ncl_: nc.gpsimd.dma_start(
            out=winb[sid_][:, :, 0:ncl_], in_=w_in_d[:, c0_:c0_ + ncl_].rearrange("(k p) n -> p k n", p=128)), w=[B_winb[sid_]])

    mk = P.mark()
    xin = [P.sb("xin%d" % i, [128, D], F32) for i in range(2)]
    B_xin = [Buf("xin%d" % i) for i in range(2)]
    xnbs = [P.sb("xnb0", [128, D], BF16)] * 2
    B_xnbs = [Buf("xnb0")] * 2
    junk = xnbs[0]
    B_junk = B_xnbs[0]
    xnT = P.sb("xnT", [128, 16, 512], BF16)
    B_xnT = Buf("xnT")
    gbc = P.sb("gbc", [128, D], F32)
    lngb = P.sb("lngb", [128, D], F32)
    B_gb = Buf("gbc")
    wsl = [P.sb("wsl%d" % i, [128, 16, 512], BF16) for i in range(3)]
    B_wsl = [Buf("wsl%d" % i) for i in range(3)]
    big = P.sb("big", [128, 4, D], F32)
    B_big = [Buf("big%d" % c) for c in range(4)]
    vn = P.sb("vn", [128, 4, D], BF16)
    B_vn = [Buf("vn%d" % c) for c in range(4)]
    qkb = P.sb("qkb", [128, 16, 512], BF16)
    B_qkb = Buf("qkb")
    qlast = P.sb("qlast", [128, 2, 16, 128], BF16)
    B_qlast = [Buf("qlast0"), Buf("qlast1")]
    tmpA = [P.sb("tmpA%d" % i, [128, 512], F32) for i in range(2)]
    B_tmpA = [Buf("tmpA%d" % i) for i in range(2)]
    tmpB = [P.sb("tmpB%d" % i, [128, 512], BF16) for i in range(3)]
    B_tmpB = [Buf("tmpB%d" % i) for i in range(3)]
    cw = [P.sb("cw%d" % i, [128, 516], F32) for i in range(2)]
    B_cw = [Buf("cw%d" % i) for i in range(2)]
    acc = [P.sb("acc%d" % i, [128, 512], F32) for i in range(2)]
    B_acc = [Buf("acc%d" % i) for i in range(2)]
    carry = P.sb("carry", [128, 32, 4], F32)
    B_carry = Buf("carry")
    convw = P.sb("convw", [128, 32, 5], F32)
    wsTb = P.sb("wsTb", [128, 8, 128], BF16)
    bsT = P.sb("bsT", [128, 8], F32)
    bcgb = P.sb("bcgb", [128, 32], F32)
    B_c1 = Buf("c1")
    sm = P.sb("sm", [128, 64], F32)
    B_sm = Buf("sm")
    st6 = P.sb("st6", [128, 4, 6], F32)
    B_st6 = Buf("st6")
    gt = P.sb("gt", [128, 4, 32], F32)
    B_gt = Buf("gt")
    pa = [P.ps("pa%d" % i, [128, 512], F32) for i in range(4)]
    B_pa = [Buf("pa%d" % i) for i in range(4)]
    ptr = P.ps("ptr", [128, 2048], BF16)
    B_ptr = Buf("ptr")
    psp = [P.ps("psp%d" % i, [128, 512], F32) for i in range(2)]
    B_psp = [Buf("psp%d" % i) for i in range(2)]

    Dsp(lambda: nc.sync.dma_start(out=gbc[:], in_=bcast_row(g_mix_d, D)), w=[B_gb])
    Dsp(lambda: nc.sync.dma_start(out=lngb[:], in_=bcast_row(ln_g_d, D)), w=[B_gb])
    Dsp(lambda: nc.sync.dma_start(out=convw[:], in_=convw_d), w=[B_c1])
    Dpl(lambda: nc.gpsimd.dma_start(out=wsTb[:], in_=wsT_d), w=[B_c1])
    Dsp(lambda: nc.sync.dma_start(out=bsT[:], in_=bsT_d), w=[B_c1])
    Dsp(lambda: nc.sync.dma_start(out=bcgb[:], in_=bcast_row(b_cg_d, 32)), w=[B_c1])
    G(lambda: nc.gpsimd.memset(carry[:], 0.0), w=[B_carry])

    st = {"w": 0, "pa": 0, "tA": 0, "tB": 0, "cw": 0}

    def load_slice(c0, ncols):
        i = st["w"] % 3
        st["w"] += 1
        Dsp(lambda: nc.sync.dma_start(out=wsl[i][:, :, 0:ncols], in_=winb[SID[c0]][:, :, 0:ncols]),
            r=[B_winb[SID[c0]]], w=[B_wsl[i]])
        return i

    def rms_rstd(src_ap, ss_col, rs_col, rd, jk=None, Bjk=None):
        jk = junk if jk is None else jk
        Bjk = B_junk if Bjk is None else Bjk
        G(lambda: nc.gpsimd.memset(ss_col, 0.0), w=[B_sm])
        A(lambda: nc.scalar.activation(out=jk[:], in_=src_ap, func=AF.Square, accum_out=ss_col),
          r=rd + [B_sm], w=[Bjk, B_sm])
        A(lambda: nc.scalar.activation(out=rs_col, in_=ss_col, func=AF.Sqrt, bias=epsb[:, 0:1], scale=1.0 / D),
          r=[B_sm, B_misc], w=[B_sm])
        V(lambda: nc.vector.reciprocal(out=rs_col, in_=rs_col), r=[B_sm], w=[B_sm])

    def transpose16(src, B_src, dst_fn, B_dst, evac_engs=("act", "dve")):
        for j in range(16):
            T(lambda j=j: nc.tensor.transpose(out=ptr[:, j * 128:(j + 1) * 128], in_=src[:, j * 128:(j + 1) * 128],
                                              identity=identb[:]), r=[B_src, B_cb], w=[B_ptr])
        for hlf in range(2):
            src_ps = ptr[:, hlf * 1024:(hlf + 1) * 1024].rearrange("p (k t) -> p k t", k=8)
            if evac_engs[hlf] == "act":
                A(lambda hlf=hlf, src_ps=src_ps: nc.scalar.copy(out=dst_fn(hlf), in_=src_ps), r=[B_ptr], w=[B_dst])
            else:
                V(lambda hlf=hlf, src_ps=src_ps: nc.vector.tensor_copy(out=dst_fn(hlf), in_=src_ps), r=[B_ptr], w=[B_dst])

    def layoutA(c0, ncols, evac):
        wi = load_slice(c0, ncols)
        for c in range(4):
            pi = st["pa"] % 4
            st["pa"] += 1
            for k in range(16):
                T(lambda k=k, c=c, pi=pi: nc.tensor.matmul(pa[pi][:, 0:ncols], lhsT=xnT[:, k, c * 128:(c + 1) * 128],
                                                          rhs=wsl[wi][:, k, 0:ncols], start=(k == 0), stop=(k == 15)),
                  r=[B_xnT, B_wsl[wi]], w=[B_pa[pi]])
            evac(c, pi)

    def write_qk_chunks(which, dst, B_dst, ti, final=False):
        j0 = ti * 4
        if not final:
            if ti > 0:
                G(lambda: nc.gpsimd.tensor_copy(out=qlast[:, which, :, 126:128], in_=qkb[:, :, 0:2]),
                  r=[B_qkb], w=[B_qlast[which]])
                Dpl(lambda: nc.gpsimd.dma_start(out=dst[j0 - 1], in_=qlast[:, which, :, :]), r=[B_qlast[which]], w=[B_dst[j0 - 1]])
            for c in range(3):
                Dpl(lambda c=c: nc.gpsimd.dma_start(out=dst[j0 + c], in_=qkb[:, :, c * 128 + 2:c * 128 + 130]),
                    r=[B_qkb], w=[B_dst[j0 + c]])
            G(lambda: nc.gpsimd.tensor_copy(out=qlast[:, which, :, 0:126], in_=qkb[:, :, 386:512]),
              r=[B_qkb], w=[B_qlast[which]])
        else:
            G(lambda: nc.gpsimd.tensor_copy(out=qlast[:, which, :, 126:128], in_=qkb[:, :, 0:2]),
              r=[B_qkb], w=[B_qlast[which]])
            Dpl(lambda: nc.gpsimd.dma_start(out=dst[NCH - 1], in_=qlast[:, which, :, :]), r=[B_qlast[which]], w=[B_dst[NCH - 1]])

    def conv_chunk(cc, sec, ci, src_ps, B_src, ncol=512):
        wi = st["cw"] % 2
        st["cw"] += 1
        V(lambda: nc.vector.tensor_copy(out=cw[wi][:, 0:4], in_=carry[:, cc, :]), r=[B_carry], w=[B_cw[wi]])
        if src_ps is not None:
            A(lambda: nc.scalar.copy(out=cw[wi][:, 4:4 + ncol], in_=src_ps), r=[B_src], w=[B_cw[wi]])
        else:
            G(lambda: nc.gpsimd.memset(cw[wi][:, 4:4 + ncol], 0.0), w=[B_cw[wi]])
        eng = V
        ev = nc.vector
        A(lambda: nc.scalar.activation(out=acc[wi][:, 0:ncol], in_=cw[wi][:, 0:ncol], func=AF.Copy, scale=convw[:, cc, 0:1]),
          r=[B_cw[wi], B_c1], w=[B_acc[wi]])
        for j in range(1, 5):
            eng(lambda j=j: ev.scalar_tensor_tensor(out=acc[wi][:, 0:ncol], in0=cw[wi][:, j:j + ncol], scalar=convw[:, cc, j:j + 1],
                                                    in1=acc[wi][:, 0:ncol], op0=ALU.mult, op1=ALU.add),
                r=[B_cw[wi], B_c1], w=[B_acc[wi]])
        if ncol == 512:
            V(lambda: nc.vector.tensor_copy(out=carry[:, cc, :], in_=cw[wi][:, 512:516]), r=[B_cw[wi]], w=[B_carry])
        if sec == 0:
            A(lambda: nc.scalar.activation(out=qkb[:, ci, 0:ncol], in_=acc[wi][:, 0:ncol], func=AF.Silu), r=[B_acc[wi]], w=[B_qkb])
        else:
            A(lambda: nc.scalar.activation(out=acc[wi][:, 0:ncol], in_=acc[wi][:, 0:ncol], func=AF.Silu), r=[B_acc[wi]], w=[B_acc[wi]])
            eng(lambda: ev.tensor_scalar(out=qkb[:, ci, 0:ncol], in0=acc[wi][:, 0:ncol], scalar1=1.0 / 16.0, scalar2=None, op0=ALU.mult),
                r=[B_acc[wi]], w=[B_qkb])

    for ti in range(NTILE):
        t0 = ti * 512
        for c in range(4):
            xi = (ti * 4 + c) % 2
            r0 = t0 + c * 128
            Dpl(lambda xi=xi, r0=r0: nc.gpsimd.dma_start(out=xin[xi][:], in_=x_d[r0:r0 + 128, :]), w=[B_xin[xi]])
            xnb, B_xnb = xnbs[xi], B_xnbs[xi]
            scol = 24 + 2 * xi
            rms_rstd(xin[xi][:], sm[:, scol:scol + 1], sm[:, scol + 1:scol + 2], [B_xin[xi]], xnb, B_xnb)
            V(lambda xi=xi, xnb=xnb, scol=scol: nc.vector.scalar_tensor_tensor(out=xnb[:], in0=xin[xi][:], scalar=sm[:, scol + 1:scol + 2], in1=gbc[:],
                                                                            op0=ALU.mult, op1=ALU.mult), r=[B_xin[xi], B_sm, B_gb], w=[B_xnb])
            transpose16(xnb, B_xnb, lambda hlf, c=c: xnT[:, hlf * 8:(hlf + 1) * 8, c * 128:(c + 1) * 128], B_xnT)

        is_ctx = ti >= NTILE_OWN
        for s in range(0 if is_ctx else 4):
            def ev_gv(c, pi, s=s):
                A(lambda: nc.scalar.activation(out=big[:, c, s * 512:(s + 1) * 512], in_=pa[pi][:], func=AF.Gelu_apprx_tanh),
                  r=[B_pa[pi]], w=[B_big[c]])
            layoutA(2048 + s * 512, 512, ev_gv)
        for c in range(0 if is_ctx else 4):
            for s in range(4):
                V(lambda c=c, s=s: nc.vector.bn_stats(out=st6[:, s, :], in_=big[:, c, s * 512:(s + 1) * 512]),
                  r=[B_big[c]], w=[B_st6])
            V(lambda: nc.vector.bn_aggr(out=sm[:, 2:4], in_=st6[:]), r=[B_st6], w=[B_sm])
            A(lambda: nc.scalar.activation(out=sm[:, 4:5], in_=sm[:, 3:4], func=AF.Sqrt, bias=epsb[:, 0:1], scale=1.0),
              r=[B_sm, B_misc], w=[B_sm])
            V(lambda: nc.vector.reciprocal(out=sm[:, 4:5], in_=sm[:, 4:5]), r=[B_sm], w=[B_sm])
            V(lambda c=c: nc.vector.tensor_scalar(out=big[:, c, :], in0=big[:, c, :], scalar1=sm[:, 2:3], scalar2=sm[:, 4:5],
                                                  op0=ALU.subtract, op1=ALU.mult), r=[B_big[c], B_sm], w=[B_big[c]])
            V(lambda c=c: nc.vector.tensor_tensor(out=vn[:, c, :], in0=big[:, c, :], in1=lngb[:], op=ALU.mult),
              r=[B_big[c], B_gb], w=[B_vn[c]])
        for s in range(4):
            def ev_v(c, pi, s=s, t0=t0):
                tb = st["tB"] % 3
                st["tB"] += 1
                A(lambda: nc.scalar.copy(out=tmpB[tb][:], in_=pa[pi][:]), r=[B_pa[pi]], w=[B_tmpB[tb]])
                j = (t0 // 128) + c
                Dpl(lambda: nc.gpsimd.dma_start(out=Vs[j * 128:(j + 1) * 128, s * 512:(s + 1) * 512], in_=tmpB[tb][:]),
                    r=[B_tmpB[tb]], w=[B_V[j]])
            layoutA(8192 + s * 512, 512, ev_v)
        for c in range(0 if is_ctx else 4):
            for g2 in range(4):
                pi = g2 % 2
                for gg in range(2):
                    g = g2 * 2 + gg
                    T(lambda c=c, g=g, gg=gg, pi=pi: nc.tensor.matmul(psp[pi][:, gg * 256:(gg + 1) * 256], lhsT=wsTb[:, g, :],
                                                                      rhs=vn[:, c, g * 256:(g + 1) * 256], start=True, stop=True),
                      r=[B_c1, B_vn[c]], w=[B_psp[pi]])
                for gg in range(2):
                    g = g2 * 2 + gg
                    V(lambda c=c, g=g, gg=gg, pi=pi: nc.vector.tensor_scalar(out=big[:, c, g * 256:(g + 1) * 256],
                                                                             in0=psp[pi][:, gg * 256:(gg + 1) * 256],
                                                                             scalar1=bsT[:, g:g + 1], scalar2=None, op0=ALU.add),
                      r=[B_psp[pi], B_c1], w=[B_big[c]])
        for s in range(0 if is_ctx else 4):
            def ev_gu(c, pi, s=s):
                ta = st["tA"] % 2
                st["tA"] += 1
                A(lambda: nc.scalar.activation(out=tmpA[ta][:], in_=pa[pi][:], func=AF.Gelu_apprx_tanh), r=[B_pa[pi]], w=[B_tmpA[ta]])
                V(lambda: nc.vector.tensor_tensor(out=big[:, c, s * 512:(s + 1) * 512], in0=tmpA[ta][:],
                                                  in1=big[:, c, s * 512:(s + 1) * 512], op=ALU.mult), r=[B_tmpA[ta], B_big[c]], w=[B_big[c]])
            layoutA(0 + s * 512, 512, ev_gu)
        for s in range(0 if is_ctx else 4):
            def ev_ga(c, pi, s=s, t0=t0):
                ta = st["tA"] % 2
                st["tA"] += 1
                tb = st["tB"] % 3
                st["tB"] += 1
                A(lambda: nc.scalar.activation(out=tmpA[ta][:], in_=pa[pi][:], func=AF.Sigmoid), r=[B_pa[pi]], w=[B_tmpA[ta]])
                V(lambda: nc.vector.tensor_tensor(out=tmpB[tb][:], in0=tmpA[ta][:], in1=big[:, c, s * 512:(s + 1) * 512], op=ALU.mult),
                  r=[B_tmpA[ta], B_big[c]], w=[B_tmpB[tb]])
                j = (t0 // 128) + c
                Dpl(lambda: nc.gpsimd.dma_start(out=A1s[j * 128:(j + 1) * 128, s * 512:(s + 1) * 512], in_=tmpB[tb][:]),
                    r=[B_tmpB[tb]], w=[B_A1[j]])
            layoutA(12320 + s * 512, 512, ev_ga)
        for s in range(0 if is_ctx else 4):
            def ev_o(c, pi, s=s):
                A(lambda: nc.scalar.activation(out=big[:, c, s * 512:(s + 1) * 512], in_=pa[pi][:], func=AF.Sigmoid),
                  r=[B_pa[pi]], w=[B_big[c]])
            layoutA(10240 + s * 512, 512, ev_o)
        for s in range(0 if is_ctx else 4):
            def ev_gb(c, pi, s=s, t0=t0):
                ta = st["tA"] % 2
                st["tA"] += 1
                tb = st["tB"] % 3
                st["tB"] += 1
                A(lambda: nc.scalar.activation(out=tmpA[ta][:], in_=pa[pi][:], func=AF.Sigmoid), r=[B_pa[pi]], w=[B_tmpA[ta]])
                V(lambda: nc.vector.tensor_tensor(out=tmpB[tb][:], in0=tmpA[ta][:], in1=big[:, c, s * 512:(s + 1) * 512], op=ALU.mult),
                  r=[B_tmpA[ta], B_big[c]], w=[B_tmpB[tb]])
                j = (t0 // 128) + c
                Dpl(lambda: nc.gpsimd.dma_start(out=G2s[j * 128:(j + 1) * 128, s * 512:(s + 1) * 512], in_=tmpB[tb][:]),
                    r=[B_tmpB[tb]], w=[B_G2[j]])
            layoutA(14368 + s * 512, 512, ev_gb)
        wi = load_slice(12288, 32)
        for c in range(4):
            pi = st["pa"] % 4
            st["pa"] += 1
            for k in range(16):
                T(lambda k=k, c=c, pi=pi, wi=wi: nc.tensor.matmul(pa[pi][:, 0:32], lhsT=xnT[:, k, c * 128:(c + 1) * 128],
                                                          rhs=wsl[wi][:, k, 0:32], start=(k == 0), stop=(k == 15)),
                  r=[B_xnT, B_wsl[wi]], w=[B_pa[pi]])
            V(lambda c=c, pi=pi: nc.vector.tensor_tensor(out=gt[:, c, :], in0=pa[pi][:, 0:32], in1=bcgb[:], op=ALU.add),
              r=[B_pa[pi], B_c1], w=[B_gt])
        gtf = gt[:].rearrange("p c (a b) -> p c a b", b=8)
        lsg = sm[:, 8:8 + 32].rearrange("p (c b) -> p c b", b=8)
        lsg2 = sm[:, 40:40 + 24]
        for a_ in (1, 3):
            src = gtf[:, :, a_, :]
            A(lambda src=src: nc.scalar.activation(out=lsg, in_=src, func=AF.Abs), r=[B_gt], w=[B_sm])
            A(lambda: nc.scalar.activation(out=lsg, in_=lsg, func=AF.Exp, scale=-1.0), r=[B_sm], w=[B_sm])
            A(lambda: nc.scalar.activation(out=lsg, in_=lsg, func=AF.Ln, bias=onec, scale=1.0), r=[B_sm, B_misc], w=[B_sm])
            V(lambda src=src: nc.vector.scalar_tensor_tensor(out=src, in0=src, scalar=0.0, in1=lsg, op0=ALU.min, op1=ALU.subtract),
              r=[B_gt, B_sm], w=[B_gt])
        for c in range(4):
            j = (t0 // 128) + c
            Dpl(lambda c=c, j=j: nc.gpsimd.dma_start(out=GAT[j * 128:(j + 1) * 128, :], in_=gt[:, c, :]), r=[B_gt], w=[B_GAT[j]])
        for sec in range(2):
            if sec == 0 and ti > NTILE_OWN - (0 if NCTX > 0 else 1):
                continue
            for s in range(4):
                wi = load_slice(4096 + sec * 2048 + s * 512, 512)
                for jj in range(4):
                    ci = s * 4 + jj
                    cc = sec * 16 + ci
                    pi = st["pa"] % 4
                    st["pa"] += 1
                    for k in range(16):
                        T(lambda k=k, jj=jj, pi=pi, wi=wi: nc.tensor.matmul(pa[pi][:, :], lhsT=wsl[wi][:, k, jj * 128:(jj + 1) * 128],
                                                                            rhs=xnT[:, k, :], start=(k == 0), stop=(k == 15)),
                          r=[B_xnT, B_wsl[wi]], w=[B_pa[pi]])
                    conv_chunk(cc, sec, ci, pa[pi][:, :], B_pa[pi])
            write_qk_chunks(sec, QT if sec == 0 else KT, B_QT if sec == 0 else B_KT, ti)
    for sec in range(1 if NCTX > 0 else 0, 2):
        for ci in range(16):
            conv_chunk(sec * 16 + ci, sec, ci, None, None, ncol=2)
        write_qk_chunks(sec, QT if sec == 0 else KT, B_QT if sec == 0 else B_KT, NTILE, final=True)
    S.flush()
    P.release(mk)

    mk = P.mark()
    qT = [P.sb("qT0", [128, 16, 128], BF16)] * 2
    kT = [P.sb("kT%d" % i, [128, 16, 128], BF16) for i in range(2)]
    vext = [P.sb("vext0", [128, 8, 257], BF16)] * 2
    gat = [P.sb("gat%d" % i, [128, 32], F32) for i in range(2)]
    B_qT = [Buf("qT0")] * 2
    B_kT = [Buf("kT%d" % i) for i in range(2)]
    B_vx = [Buf("vx0")] * 2
    B_gat = [Buf("gat%d" % i) for i in range(2)]
    ktok = P.sb("ktok", [128, D], BF16)
    B_ktok = Buf("ktok")
    Cst = P.sb("Cst", [128, 8, 2, 257], F32)
    Cb = P.sb("Cb", [128, 8, 2, 257], BF16)
    B_C = [Buf("C%d" % h) for h in range(8)]
    B_Cb = [Buf("Cb%d" % h) for h in range(8)]
    mrow = P.sb("mrow", [8, 1], F32)
    B_mrow = Buf("mrow")
    rw = P.sb("rw", [8, 640], F32)
    B_rw = Buf("rw")
    dg = P.sb("dg", [8, 24], F32)
    B_dg = Buf("dg")
    cl = P.sb("cl", [128, 128], F32)
    B_cl = Buf("cl")
    B_den = Buf("den")
    Et = [P.sb("Et%d" % i, [128, 128], F32) for i in range(2)]
    B_Et = [Buf("Et%d" % i) for i in range(2)]
    SmT = [P.sb("SmT%d" % i, [128, 128], BF16) for i in range(2)]
    B_SmT = [Buf("SmT%d" % i) for i in range(2)]
    nint = [P.sb("nint%d" % i, [128, 257], F32) for i in range(2)]
    B_nint = [Buf("nint%d" % i) for i in range(2)]
    numt = [P.sb("numt%d" % i, [128, 257], F32) for i in range(2)]
    B_numt = [Buf("numt%d" % i) for i in range(2)]
    kw = [P.sb("kw%d" % i, [128, 256], BF16) for i in range(2)]
    B_kw = [Buf("kw%d" % i) for i in range(2)]
    hraw = P.sb("hraw", [128, 8, 256], F32)
    B_hraw = Buf("hraw")
    hout = P.sb("hout", [128, D], F32)
    B_hout = Buf("hout")
    hfin = P.sb("hfinb", [128, D], F32)
    B_hfin = Buf("hfinb")
    g2in = P.sb("g2in", [128, D], BF16)
    a1in = P.sb("a1in", [128, D], BF16)
    B_ga = Buf("g2a1")
    hgb = P.sb("hgb", [128, D], F32)
    g2bc = P.sb("g2bc", [128, D], F32)
    B_hg = Buf("hgb")
    mixb = P.sb("mixb", [128, D], BF16)
    B_mixb = Buf("mixb")
    mixT = P.sb("mixT", [128, 16, 128], BF16)
    B_mixT = Buf("mixT")
    woutb = P.sb("woutb", [128, 16, D], BF16)
    B_wout = Buf("woutb")
    x3 = P.sb("x3", [128, D], F32)
    B_x3 = Buf("x3")
    h2b = P.sb("h2b", [128, D], BF16)
    B_h2b = Buf("h2b")
    junk = h2b
    B_junk = B_h2b
    wrb = P.sb("wrb", [128, 16, 36], BF16)
    brb = P.sb("brb", [128, 36], F32)
    B_wr = Buf("wrb")
    rt = P.sb("rt", [128, 352], F32)
    B_rt = Buf("rt")
    cnt = P.sb("cnt", [128, 32], F32)
    B_cnt = Buf("cnt")
    Mb = P.sb("Mb", [128, 32], BF16)
    B_Mb = Buf("Mb")
    tokid = P.sb("tokid", [128, 1], I32)
    B_tokid = Buf("tokid")
    zrow = junk
    B_z = Buf("z")
    sm = P.sb("sm3", [128, 64], F32)
    B_sm = Buf("sm3")

    pS = P.ps("pS", [128, 512], F32)
    B_pS = [Buf("pS%d" % i) for i in range(4)]
    pE = P.ps("pE", [128, 512], F32)
    B_pE = [Buf("pE%d" % i) for i in range(4)]
    pN = [P.ps("pN0", [128, 512], F32)] * 2
    B_pN = [Buf("pN0")] * 2
    pQ = [P.ps("pQ0", [128, 512], F32)] * 2
    B_pQ = [Buf("pQ0")] * 2
    ptr = P.ps("ptr3", [128, 2048], BF16)
    B_ptr = Buf("ptr3")
    pX = P.ps("pX", [128, 1024], F32)
    B_pX = Buf("pX")

    Dsp(lambda: nc.sync.dma_start(out=hgb[:], in_=bcast_row(headg_d, D)), w=[B_hg])
    Dsp(lambda: nc.sync.dma_start(out=g2bc[:], in_=bcast_row(g_ffn_d, D)), w=[B_hg])
    Dsp(lambda: nc.sync.dma_start(out=brb[:], in_=bcast_row(b_r_d, 36)), w=[B_wr])
    Dpl(lambda: nc.gpsimd.dma_start(out=wrb[:], in_=w_r_d.rearrange("(k p) n -> p k n", p=128)), w=[B_wr])
    for k4 in range(4):
        Dpl(lambda k4=k4: nc.gpsimd.dma_start(out=woutb[:, k4 * 4:(k4 + 1) * 4, :],
                                              in_=w_out_d[k4 * 512:(k4 + 1) * 512, :].rearrange("(k p) n -> p k n", p=128)),
            w=[B_wout])
    G(lambda: nc.gpsimd.memset(vext[0][:, :, 256:257], 1.0), w=[B_vx[0]])
    G(lambda: nc.gpsimd.memset(cnt[:], 0.0), w=[B_cnt])
    G(lambda: nc.gpsimd.memset(zrow[:], 0.0), w=[B_z, B_junk])
    G(lambda: nc.gpsimd.memset(x3[0:1, :], 0.0), w=[B_x3])
    Dsp(lambda: nc.sync.dma_start(out=YsA[NLh:NLh + 1, :], in_=x3[0:1, :]), r=[B_x3], w=[B_Y])
    Dsp(lambda: nc.sync.dma_start(out=YsB[NLh:NLh + 1, :], in_=x3[0:1, :]), r=[B_x3], w=[B_Y])
    G(lambda: nc.gpsimd.tensor_copy(out=rt[:, 300:340], in_=rcst[:, 0:40]), r=[B_misc], w=[B_rt])
    Dsp(lambda: nc.sync.dma_start(out=H2s[NTO:NTO + 128, :], in_=zrow[:]), r=[B_z, B_junk], w=[B_H2])
    Dsp(lambda: nc.sync.dma_start(out=Ls.rearrange("(p n) o -> p (n o)", p=128), in_=padi_d), w=[B_L])

    def bf_view(ps_tile, lo, n):
        return ps_tile[:, lo:lo + n // 2].bitcast(BF16)

    def mlstm_loads(j, bi, scan_only=False):
        q_, k_, v_, g_ = qT[bi], kT[bi], vext[bi], gat[bi]
        Bq, Bk, Bv, Bg = B_qT[bi], B_kT[bi], B_vx[bi], B_gat[bi]
        Dsp(lambda: nc.sync.dma_start(out=k_[:], in_=KT[j]), r=[B_KT[j]], w=[Bk])
        Dsp(lambda: nc.sync.dma_start(out=g_[:], in_=GAT[j * 128:(j + 1) * 128, :]), r=[B_GAT[j]], w=[Bg])

    def mlstm_loads_qv(j, bi, scan_only=False):
        q_, v_ = qT[bi], vext[bi]
        Bq, Bv = B_qT[bi], B_vx[bi]
        if not scan_only:
            Dsp(lambda: nc.sync.dma_start(out=q_[:], in_=QT[j]), r=[B_QT[j]], w=[Bq])
        Dsp(lambda: nc.sync.dma_start(out=v_[:, :, 0:256], in_=Vs[j * 128:(j + 1) * 128, :].rearrange("p (h e) -> p h e", h=8)),
            r=[B_V[j]], w=[Bv])

    def mlstm_chunk(j, bwd, bi, scan_only=False, filler=None):
        def adv():
            if filler is not None:
                next(filler, None)
        mlstm_loads_qv(j, bi, scan_only)
        Tm, MA, MAT = (T_b, MA_b, MAT_b) if bwd else (T_f, MA_f, MAT_f)
        gi, gf = (16, 24) if bwd else (0, 8)
        last = 0 if bwd else 127
        q_, k_, v_, g_ = qT[bi], kT[bi], vext[bi], gat[bi]
        Bq, Bk, Bv, Bg = B_qT[bi], B_kT[bi], B_vx[bi], B_gat[bi]
        li = g_[:, gi:gi + 8]
        lf = g_[:, gf:gf + 8]
        for c in range(16):
            T(lambda c=c: nc.tensor.transpose(out=ptr[:, c * 128:(c + 1) * 128], in_=k_[:, c, :], identity=identb[:]),
              r=[Bk, B_cb], w=[B_ptr])
        A(lambda: nc.scalar.copy(out=ktok[:, 0:1024], in_=ptr[:, 0:1024]), r=[B_ptr], w=[B_ktok])
        V(lambda: nc.vector.tensor_copy(out=ktok[:, 1024:2048], in_=ptr[:, 1024:2048]), r=[B_ptr], w=[B_ktok])
        T(lambda: nc.tensor.matmul(pX[:, 0:8], lhsT=Tm, rhs=lf, start=True, stop=True), r=[B_cst, Bg], w=[B_pX])
        T(lambda: nc.tensor.matmul(pX[0:8, 64:192], lhsT=lf, rhs=Tm, start=True, stop=True), r=[B_cst, Bg], w=[B_pX])
        T(lambda: nc.tensor.matmul(pX[0:8, 192:320], lhsT=li, rhs=identf, start=True, stop=True), r=[B_cst, Bg], w=[B_pX])
        Frow, urow = rw[:, 0:128], rw[:, 128:256]
        A(lambda: nc.scalar.copy(out=Frow, in_=pX[0:8, 64:192]), r=[B_pX], w=[B_rw])
        V(lambda: nc.vector.tensor_tensor(out=urow, in0=pX[0:8, 192:320], in1=Frow, op=ALU.subtract), r=[B_pX, B_rw], w=[B_rw])
        umax, FLc, t1, mnew, d1, arow, d2 = (rw[:, 512 + i:513 + i] for i in range(7))
        V(lambda: nc.vector.tensor_reduce(out=umax, in_=urow, axis=AX.X, op=ALU.max), r=[B_rw], w=[B_rw])
        V(lambda: nc.vector.tensor_copy(out=FLc, in_=rw[:, last:last + 1]), r=[B_rw], w=[B_rw])
        V(lambda: nc.vector.tensor_tensor(out=t1, in0=mrow[:, 0:1], in1=umax, op=ALU.max), r=[B_rw, B_mrow], w=[B_rw])
        V(lambda: nc.vector.tensor_tensor(out=mnew, in0=FLc, in1=t1, op=ALU.add), r=[B_rw], w=[B_rw])
        V(lambda: nc.vector.tensor_tensor(out=d1, in0=FLc, in1=mrow[:, 0:1], op=ALU.add), r=[B_rw, B_mrow], w=[B_rw])
        V(lambda: nc.vector.tensor_tensor(out=d1, in0=d1, in1=mnew, op=ALU.subtract), r=[B_rw], w=[B_rw])
        A(lambda: nc.scalar.activation(out=arow, in_=d1, func=AF.Exp), r=[B_rw], w=[B_rw])
        V(lambda: nc.vector.tensor_tensor(out=d2, in0=FLc, in1=mnew, op=ALU.subtract), r=[B_rw], w=[B_rw])
        V(lambda: nc.vector.tensor_scalar(out=dg[:, 0:8], in0=ident8, scalar1=d2, scalar2=None, op0=ALU.mult), r=[B_sel, B_rw], w=[B_dg])
        V(lambda: nc.vector.tensor_scalar(out=dg[:, 8:16], in0=ident8, scalar1=mrow[:, 0:1], scalar2=None, op0=ALU.mult),
          r=[B_sel, B_mrow], w=[B_dg])
        V(lambda: nc.vector.tensor_scalar(out=dg[:, 16:24], in0=ident8, scalar1=arow, scalar2=None, op0=ALU.mult), r=[B_sel, B_rw], w=[B_dg])
        T(lambda: nc.tensor.matmul(pX[:, 8:32], lhsT=ones8, rhs=dg[:, :], start=True, stop=True), r=[B_sel, B_dg], w=[B_pX])
        A(lambda: nc.scalar.copy(out=cl[:, 0:32], in_=pX[:, 0:32]), r=[B_pX], w=[B_cl])
        Fc, d2bc, mpbc, abc, ucol, kwsc, mxu, Gc, wint, enm, den8, rden, tmpc = (cl[:, 8 * i:8 * i + 8] for i in range(13))
        V(lambda: nc.vector.tensor_tensor(out=ucol, in0=li, in1=Fc, op=ALU.subtract), r=[Bg, B_cl], w=[B_cl])
        V(lambda: nc.vector.tensor_tensor(out=kwsc, in0=ucol, in1=d2bc, op=ALU.add), r=[B_cl], w=[B_cl])
        A(lambda: nc.scalar.activation(out=kwsc, in_=kwsc, func=AF.Exp), r=[B_cl], w=[B_cl])
        for h in range(0 if scan_only else 8):
            T(lambda h=h: nc.tensor.matmul(pXd(h), lhsT=selt[:, h * 128:(h + 1) * 128],
                                           rhs=urow, start=True, stop=False), r=[B_sel, B_rw, B_cl], w=[B_pX])
            T(lambda h=h: nc.tensor.matmul(pXd(h), lhsT=identf, rhs=MA, start=False, stop=True), r=[B_cst], w=[B_pX])
        if not scan_only:
            V(lambda: nc.vector.tensor_reduce(out=mxu, in_=pX[:, 0:1024].rearrange("p (h s) -> p h s", h=8), axis=AX.X, op=ALU.max),
              r=[B_pX], w=[B_cl])
            V(lambda: nc.vector.tensor_tensor(out=Gc, in0=mpbc, in1=mxu, op=ALU.max), r=[B_cl], w=[B_cl])
            V(lambda: nc.vector.tensor_tensor(out=wint, in0=mpbc, in1=Gc, op=ALU.subtract), r=[B_cl], w=[B_cl])
            A(lambda: nc.scalar.activation(out=wint, in_=wint, func=AF.Exp), r=[B_cl], w=[B_cl])
            V(lambda: nc.vector.tensor_tensor(out=enm, in0=Fc, in1=Gc, op=ALU.add), r=[B_cl], w=[B_cl])
            A(lambda: nc.scalar.activation(out=enm, in_=enm, func=AF.Exp, scale=-1.0), r=[B_cl], w=[B_cl])
            T(lambda: nc.tensor.matmul(pX[0:8, 0:128], lhsT=Gc, rhs=identf, start=True, stop=True), r=[B_cl, B_cst], w=[B_pX])
            nGrow = rw[:, 256:384]
            A(lambda: nc.scalar.activation(out=nGrow, in_=pX[0:8, 0:128], func=AF.Copy, scale=-1.0), r=[B_pX], w=[B_rw])
        adv()
        adv()
        for h in range(8):
            r4 = h % 4
            i2 = h % 2
            psS = pS[:, r4 * 128:(r4 + 1) * 128]
            psE = pE[:, r4 * 128:(r4 + 1) * 128]
            if not scan_only:
                for dc in range(2):
                    T(lambda h=h, dc=dc, psS=psS: nc.tensor.matmul(psS, lhsT=k_[:, 2 * h + dc, :], rhs=q_[:, 2 * h + dc, :],
                                                                   start=(dc == 0), stop=(dc == 1)), r=[Bq, Bk], w=[B_pS[r4]])
                T(lambda h=h, psE=psE: nc.tensor.matmul(psE, lhsT=selt[:, h * 128:(h + 1) * 128], rhs=nGrow, start=True, stop=False),
                  r=[B_sel, B_rw], w=[B_pE[r4]])
                T(lambda psE=psE: nc.tensor.matmul(psE, lhsT=identf, rhs=MAT, start=False, stop=True), r=[B_cst], w=[B_pE[r4]])
                A(lambda h=h, psE=psE, i2=i2: nc.scalar.activation(out=Et[i2][:], in_=psE, func=AF.Exp, bias=ucol[:, h:h + 1], scale=1.0),
                  r=[B_pE[r4], B_cl], w=[B_Et[i2]])
                V(lambda psS=psS, i2=i2: nc.vector.tensor_tensor(out=SmT[i2][:], in0=psS, in1=Et[i2][:], op=ALU.mult),
                  r=[B_pS[r4], B_Et[i2]], w=[B_SmT[i2]])
                T(lambda h=h, i2=i2: nc.tensor.matmul(pN[i2][:, 0:257], lhsT=SmT[i2][:], rhs=v_[:, h, :], start=True, stop=True),
                  r=[B_SmT[i2], Bv], w=[B_pN[i2]])
                for dc in range(2):
                    T(lambda h=h, dc=dc, i2=i2: nc.tensor.matmul(pQ[i2][:, 0:257], lhsT=q_[:, 2 * h + dc, :], rhs=Cb[:, h, dc, :],
                                                                 start=(dc == 0), stop=(dc == 1)), r=[Bq, B_Cb[h]], w=[B_pQ[i2]])
                A(lambda i2=i2: nc.scalar.copy(out=nint[i2][:], in_=pN[i2][:, 0:257]), r=[B_pN[i2]], w=[B_nint[i2]])
                V(lambda h=h, i2=i2: nc.vector.scalar_tensor_tensor(out=hraw[:, h, :], in0=pQ[i2][:, 0:256], scalar=wint[:, h:h + 1],
                                                                    in1=nint[i2][:, 0:256], op0=ALU.mult, op1=ALU.add),
                  r=[B_pQ[i2], B_cl, B_nint[i2]], w=[B_hraw])
                V(lambda h=h, i2=i2: nc.vector.scalar_tensor_tensor(out=den8[:, h:h + 1], in0=pQ[i2][:, 256:257], scalar=wint[:, h:h + 1],
                                                                    in1=nint[i2][:, 256:257], op0=ALU.mult, op1=ALU.add),
                  r=[B_pQ[i2], B_cl, B_nint[i2]], w=[B_den])
            A(lambda h=h, i2=i2: nc.scalar.activation(out=kw[i2][:], in_=ktok[:, h * 256:(h + 1) * 256], func=AF.Copy,
                                                      scale=kwsc[:, h:h + 1]), r=[B_ktok, B_cl], w=[B_kw[i2]])
            for dc in range(2):
                T(lambda h=h, dc=dc, i2=i2: nc.tensor.matmul(pXc(dc), lhsT=kw[i2][:, dc * 128:(dc + 1) * 128], rhs=v_[:, h, :],
                                                             start=True, stop=True), r=[B_kw[i2], Bv, B_cl, B_rw], w=[B_pX])
            for dc in range(2):
                V(lambda h=h, dc=dc: nc.vector.scalar_tensor_tensor(out=Cst[:, h, dc, :], in0=Cst[:, h, dc, :], scalar=abc[:, h:h + 1],
                                                                    in1=pXc(dc), op0=ALU.mult, op1=ALU.add),
                  r=[B_C[h], B_cl, B_pX], w=[B_C[h]])
            A(lambda h=h: nc.scalar.copy(out=Cb[:, h, :, :], in_=Cst[:, h, :, :]), r=[B_C[h]], w=[B_Cb[h]])
            adv()
            adv()
        V(lambda: nc.vector.tensor_copy(out=mrow[:, 0:1], in_=mnew), r=[B_rw, B_dg], w=[B_mrow])
        if filler is not None:
            for _ in filler:
                pass
        if scan_only:
            return None
        A(lambda: nc.scalar.activation(out=tmpc, in_=den8, func=AF.Abs), r=[B_cl, B_den], w=[B_cl])
        V(lambda: nc.vector.tensor_tensor(out=tmpc, in0=tmpc, in1=enm, op=ALU.max), r=[B_cl], w=[B_cl])
        V(lambda: nc.vector.reciprocal(out=rden, in_=tmpc), r=[B_cl], w=[B_cl])
        return rden

    def pXd(h):
        return pX[:, h * 128:(h + 1) * 128]

    def pXc(dc):
        return pX[:, dc * 512:dc * 512 + 257]

    def reset_state(full):
        if full:
            G(lambda: nc.gpsimd.memset(Cst[:], 0.0), w=B_C)
            G(lambda: nc.gpsimd.memset(Cb[:], 0.0), w=B_Cb)
            G(lambda: nc.gpsimd.memset(mrow[:], 0.0), w=[B_mrow])
        else:
            G(lambda: nc.gpsimd.tensor_scalar(out=Cst[:], in0=Cst[:], scalar1=keept[:, 0:1], scalar2=None, op0=ALU.mult),
              r=B_C + [B_misc], w=B_C)
            G(lambda: nc.gpsimd.tensor_scalar(out=Cb[:], in0=Cb[:], scalar1=keept[:, 0:1], scalar2=None, op0=ALU.mult),
              r=B_Cb + [B_misc], w=B_Cb)
            G(lambda: nc.gpsimd.tensor_scalar(out=mrow[:], in0=mrow[:], scalar1=keept[0:8, 0:1], scalar2=None, op0=ALU.mult),
              r=[B_mrow, B_misc], w=[B_mrow])

    reset_state(True)
    mlstm_loads(0, 0)
    for n_, j in enumerate(range(NOWN)):
        issue_casts(casts_per_iter)
        if j + 1 < NOWN:
            mlstm_loads(j + 1, (n_ + 1) % 2)
        rden = mlstm_chunk(j, False, n_ % 2)
        for h in range(8):
            if h % 2:
                A(lambda h=h, rden=rden: nc.scalar.activation(out=hout[:, h * 256:(h + 1) * 256], in_=hraw[:, h, :], func=AF.Copy,
                                                              scale=rden[:, h:h + 1]), r=[B_hraw, B_cl], w=[B_hout])
            else:
                V(lambda h=h, rden=rden: nc.vector.tensor_scalar(out=hout[:, h * 256:(h + 1) * 256], in0=hraw[:, h, :], scalar1=rden[:, h:h + 1],
                                                                 scalar2=None, op0=ALU.mult), r=[B_hraw, B_cl], w=[B_hout])
        Dpl(lambda j=j: nc.gpsimd.dma_start(out=HFs[j * 128:(j + 1) * 128, :], in_=hout[:]), r=[B_hout], w=[B_HF[j]])
    S.flush()

    reset_state(True)
    def merge_gen(j):
        Dsp(lambda j=j: nc.sync.dma_start(out=g2in[:], in_=G2s[j * 128:(j + 1) * 128, :]), r=[B_G2[j]], w=[B_ga])
        Dsp(lambda j=j: nc.sync.dma_start(out=a1in[:], in_=A1s[j * 128:(j + 1) * 128, :]), r=[B_A1[j]], w=[B_ga])
        Dsp(lambda j=j: nc.sync.dma_start(out=x3[:], in_=x_d[j * 128:(j + 1) * 128, :]), w=[B_x3])
        G(lambda: nc.gpsimd.memset(sm[:, 0:8], 0.0), w=[B_sm])
        for h in range(8):
            A(lambda h=h: nc.scalar.activation(out=junk[:, 0:256], in_=hout[:, h * 256:(h + 1) * 256], func=AF.Square,
                                               accum_out=sm[:, h:h + 1]), r=[B_hout, B_sm], w=[B_junk, B_sm])
        yield
        A(lambda: nc.scalar.activation(out=sm[:, 8:16], in_=sm[:, 0:8], func=AF.Sqrt, bias=epsb[:, 0:1], scale=1.0 / 256.0),
          r=[B_sm, B_misc], w=[B_sm])
        V(lambda: nc.vector.reciprocal(out=sm[:, 8:16], in_=sm[:, 8:16]), r=[B_sm], w=[B_sm])
        for h in range(8):
            eng, ev = (V, nc.vector)
            eng(lambda h=h, ev=ev: ev.scalar_tensor_tensor(out=hout[:, h * 256:(h + 1) * 256], in0=hout[:, h * 256:(h + 1) * 256],
                                                           scalar=sm[:, 8 + h:9 + h], in1=hgb[:, h * 256:(h + 1) * 256],
                                                           op0=ALU.mult, op1=ALU.mult), r=[B_hout, B_sm, B_hg], w=[B_hout])
        yield
        V(lambda: nc.vector.tensor_tensor(out=hout[:], in0=hout[:], in1=g2in[:], op=ALU.mult), r=[B_hout, B_ga], w=[B_hout])
        V(lambda: nc.vector.tensor_tensor(out=mixb[:], in0=hout[:], in1=a1in[:], op=ALU.add), r=[B_hout, B_ga], w=[B_mixb])
        yield
        for c in range(16):
            T(lambda c=c: nc.tensor.transpose(out=ptr[:, c * 128:(c + 1) * 128], in_=mixb[:, c * 128:(c + 1) * 128], identity=identb[:]),
              r=[B_mixb, B_cb], w=[B_ptr])
        A(lambda: nc.scalar.copy(out=mixT[:, 0:8, :], in_=ptr[:, 0:1024].rearrange("p (k t) -> p k t", k=8)), r=[B_ptr], w=[B_mixT])
        V(lambda: nc.vector.tensor_copy(out=mixT[:, 8:16, :], in_=ptr[:, 1024:2048].rearrange("p (k t) -> p k t", k=8)), r=[B_ptr], w=[B_mixT])
        yield
        obanks = [(pN[0], B_pN[0]), (pQ[0], B_pQ[0]), (pN[0], B_pN[0]), (pQ[0], B_pQ[0])]
        for ns in range(4):
            pb, Bpb = obanks[ns]
            for k in range(16):
                T(lambda k=k, ns=ns, pb=pb: nc.tensor.matmul(pb[:, :], lhsT=mixT[:, k, :], rhs=woutb[:, k, ns * 512:(ns + 1) * 512],
                                                             start=(k == 0), stop=(k == 15)), r=[B_mixT, B_wout], w=[Bpb])
            V(lambda ns=ns, pb=pb: nc.vector.tensor_tensor(out=x3[:, ns * 512:(ns + 1) * 512], in0=x3[:, ns * 512:(ns + 1) * 512],
                                                           in1=pb[:, :], op=ALU.add), r=[B_x3, Bpb], w=[B_x3])
            yield
        Dpl(lambda j=j: nc.gpsimd.dma_start(out=X1s[j * 128:(j + 1) * 128, :], in_=x3[:]), r=[B_x3], w=[B_X1[j]])
        yield
        G(lambda: nc.gpsimd.memset(sm[:, 16:17], 0.0), w=[B_sm])
        A(lambda: nc.scalar.activation(out=junk[:], in_=x3[:], func=AF.Square, accum_out=sm[:, 16:17]), r=[B_x3, B_sm], w=[B_junk, B_sm])
        A(lambda: nc.scalar.activation(out=sm[:, 17:18], in_=sm[:, 16:17], func=AF.Sqrt, bias=epsb[:, 0:1], scale=1.0 / D),
          r=[B_sm, B_misc], w=[B_sm])
        V(lambda: nc.vector.reciprocal(out=sm[:, 17:18], in_=sm[:, 17:18]), r=[B_sm], w=[B_sm])
        V(lambda: nc.vector.scalar_tensor_tensor(out=h2b[:], in0=x3[:], scalar=sm[:, 17:18], in1=g2bc[:], op0=ALU.mult, op1=ALU.mult),
          r=[B_x3, B_sm, B_hg], w=[B_h2b])
        Dpl(lambda j=j: nc.gpsimd.dma_start(out=H2s[j * 128:(j + 1) * 128, :], in_=h2b[:]), r=[B_h2b], w=[B_H2])
        yield
        for c in range(16):
            T(lambda c=c: nc.tensor.transpose(out=ptr[:, c * 128:(c + 1) * 128], in_=h2b[:, c * 128:(c + 1) * 128], identity=identb[:]),
              r=[B_h2b, B_cb], w=[B_ptr])
        A(lambda: nc.scalar.copy(out=mixT[:, 0:8, :], in_=ptr[:, 0:1024].rearrange("p (k t) -> p k t", k=8)), r=[B_ptr], w=[B_mixT])
        V(lambda: nc.vector.tensor_copy(out=mixT[:, 8:16, :], in_=ptr[:, 1024:2048].rearrange("p (k t) -> p k t", k=8)), r=[B_ptr], w=[B_mixT])
        for k in range(16):
            T(lambda k=k: nc.tensor.matmul(pX[:, 0:36], lhsT=mixT[:, k, :], rhs=wrb[:, k, :], start=(k == 0), stop=(k == 15)),
              r=[B_mixT, B_wr], w=[B_pX])
        yield
        lg = rt[:, 0:36]
        V(lambda: nc.vector.tensor_tensor(out=lg, in0=pX[:, 0:36], in1=brb[:], op=ALU.add), r=[B_pX, B_wr], w=[B_rt])
        gmax, gsum, pg, v1, v2, wk1, wk2, dsum = (rt[:, 40 + i:41 + i] for i in range(8))
        ohg = rt[:, 48:52]
        eg = rt[:, 52:56]
        msk = rt[:, 64:96]
        oh1 = rt[:, 96:128]
        oh2 = rt[:, 128:160]
        Mt = rt[:, 160:192]
        pos = rt[:, 192:224]
        tmp32 = rt[:, 224:256]
        ebase = rt[:, 256:288]
        dst = rt[:, 288:290]
        dst6 = rt[:, 288:294]
        isA = rt[:, 340:342]
        isB = rt[:, 342:344]
        V(lambda: nc.vector.tensor_reduce(out=gmax, in_=lg[:, 0:4], axis=AX.X, op=ALU.max), r=[B_rt], w=[B_rt])
        V(lambda: nc.vector.tensor_scalar(out=ohg, in0=lg[:, 0:4], scalar1=gmax, scalar2=None, op0=ALU.is_equal), r=[B_rt], w=[B_rt])
        V(lambda: nc.vector.tensor_scalar(out=eg, in0=lg[:, 0:4], scalar1=gmax, scalar2=None, op0=ALU.subtract), r=[B_rt], w=[B_rt])
        A(lambda: nc.scalar.activation(out=eg, in_=eg, func=AF.Exp), r=[B_rt], w=[B_rt])
        V(lambda: nc.vector.tensor_reduce(out=gsum, in_=eg, axis=AX.X, op=ALU.add), r=[B_rt], w=[B_rt])
        V(lambda: nc.vector.reciprocal(out=pg, in_=gsum), r=[B_rt], w=[B_rt])
        yield
        for g in range(4):
            V(lambda g=g: nc.vector.tensor_scalar(out=msk[:, g * 8:(g + 1) * 8], in0=lg[:, 4 + g * 8:12 + g * 8], scalar1=ohg[:, g:g + 1],
                                                  scalar2=None, op0=ALU.mult), r=[B_rt], w=[B_rt])
            V(lambda g=g: nc.vector.tensor_scalar(out=tmp32[:, g * 8:(g + 1) * 8], in0=rt[:, 300:308],
                                                  scalar1=ohg[:, g:g + 1], scalar2=None, op0=ALU.mult), r=[B_rt], w=[B_rt])
        V(lambda: nc.vector.tensor_scalar(out=tmp32, in0=tmp32, scalar1=-1.0, scalar2=1e4, op0=ALU.add, op1=ALU.mult), r=[B_rt], w=[B_rt])
        V(lambda: nc.vector.tensor_tensor(out=msk, in0=msk, in1=tmp32, op=ALU.add), r=[B_rt], w=[B_rt])
        V(lambda: nc.vector.tensor_reduce(out=v1, in_=msk, axis=AX.X, op=ALU.max), r=[B_rt], w=[B_rt])
        V(lambda: nc.vector.tensor_scalar(out=oh1, in0=msk, scalar1=v1, scalar2=None, op0=ALU.is_equal), r=[B_rt], w=[B_rt])
        V(lambda: nc.vector.scalar_tensor_tensor(out=tmp32, in0=oh1, scalar=-1e4, in1=msk, op0=ALU.mult, op1=ALU.add), r=[B_rt], w=[B_rt])
        V(lambda: nc.vector.tensor_reduce(out=v2, in_=tmp32, axis=AX.X, op=ALU.max), r=[B_rt], w=[B_rt])
        V(lambda: nc.vector.tensor_scalar(out=oh2, in0=tmp32, scalar1=v2, scalar2=None, op0=ALU.is_equal), r=[B_rt], w=[B_rt])
        yield
        V(lambda: nc.vector.tensor_tensor(out=dsum, in0=v1, in1=v2, op=ALU.subtract), r=[B_rt], w=[B_rt])
        A(lambda: nc.scalar.activation(out=dsum, in_=dsum, func=AF.Sigmoid), r=[B_rt], w=[B_rt])
        V(lambda j=j: nc.vector.tensor_tensor(out=w_all[:, j, 0:1], in0=pg, in1=dsum, op=ALU.mult), r=[B_rt], w=[B_dest[j]])
        V(lambda j=j: nc.vector.tensor_tensor(out=w_all[:, j, 1:2], in0=pg, in1=w_all[:, j, 0:1], op=ALU.subtract), r=[B_rt, B_dest[j]], w=[B_dest[j]])
        yield
        V(lambda j=j: nc.vector.tensor_scalar(out=oh1, in0=oh1, scalar1=validt[:, j:j + 1], scalar2=None, op0=ALU.mult), r=[B_rt, B_misc], w=[B_rt])
        V(lambda j=j: nc.vector.tensor_scalar(out=oh2, in0=oh2, scalar1=validt[:, j:j + 1], scalar2=None, op0=ALU.mult), r=[B_rt, B_misc], w=[B_rt])
        V(lambda: nc.vector.tensor_tensor(out=Mt, in0=oh1, in1=oh2, op=ALU.add), r=[B_rt], w=[B_rt])
        V(lambda: nc.vector.tensor_copy(out=Mb[:], in_=Mt), r=[B_rt], w=[B_Mb])
        T(lambda: nc.tensor.matmul(pX[:, 64:96], lhsT=tristb[:], rhs=Mb[:], start=True, stop=True), r=[B_cb, B_Mb], w=[B_pX])
        T(lambda: nc.tensor.matmul(pX[:, 96:128], lhsT=onesb[:], rhs=Mb[:], start=True, stop=True), r=[B_cb, B_Mb], w=[B_pX])
        V(lambda: nc.vector.tensor_tensor(out=pos, in0=pX[:, 64:96], in1=cnt[:], op=ALU.add), r=[B_pX, B_cnt], w=[B_rt])
        V(lambda: nc.vector.tensor_tensor(out=cnt[:], in0=cnt[:], in1=pX[:, 96:128], op=ALU.add), r=[B_pX, B_cnt], w=[B_cnt])
        for kk, ohk in enumerate((oh1, oh2)):
            V(lambda ohk=ohk: nc.vector.tensor_tensor(out=tmp32, in0=pos, in1=ohk, op=ALU.mult), r=[B_rt], w=[B_rt])
            V(lambda kk=kk, j=j: nc.vector.tensor_reduce(out=pe_all[:, j, kk:kk + 1], in_=tmp32, axis=AX.X, op=ALU.add), r=[B_rt], w=[B_dest[j]])
            V(lambda ohk=ohk: nc.vector.tensor_tensor(out=tmp32, in0=iota32, in1=ohk, op=ALU.mult), r=[B_rt, B_misc], w=[B_rt])
            V(lambda kk=kk, j=j: nc.vector.tensor_reduce(out=pe_all[:, j, 2 + kk:3 + kk], in_=tmp32, axis=AX.X, op=ALU.add), r=[B_rt], w=[B_dest[j]])
        yield

    mlstm_loads(NCH - 1, 0, scan_only=(NCTX > 0))
    pending = None
    for n_, j in enumerate(range(NCH - 1, -1, -1)):
        issue_casts(casts_per_iter)
        if NCTX > 0 and j == NOWN - 1:
            reset_state(False)
        if j - 1 >= 0:
            mlstm_loads(j - 1, (n_ + 1) % 2, scan_only=(j - 1 >= NOWN))
        if j >= NOWN:
            mlstm_chunk(j, True, n_ % 2, scan_only=True)
            continue
        Dsp(lambda j=j: nc.sync.dma_start(out=hfin[:], in_=HFs[j * 128:(j + 1) * 128, :]), r=[B_HF[j]], w=[B_hfin])
        rden = mlstm_chunk(j, True, n_ % 2, filler=pending)
        for h in range(8):
            V(lambda h=h, rden=rden: nc.vector.scalar_tensor_tensor(out=hout[:, h * 256:(h + 1) * 256], in0=hraw[:, h, :],
                                                                    scalar=rden[:, h:h + 1], in1=hfin[:, h * 256:(h + 1) * 256],
                                                                    op0=ALU.mult, op1=ALU.add), r=[B_hraw, B_cl, B_hfin], w=[B_hout])
        pending = merge_gen(j)
    if pending is not None:
        for _ in pending:
            pass
    issue_casts(len(cast_jobs))
    nblk = rt[:, 0:32]
    incl = rt[:, 32:64]
    t32 = rt[:, 64:96]
    bacc = rt[:, 96:224]
    t128 = rt[:, 224:352]
    V(lambda: nc.vector.memset(nblk, 0.0), w=[B_rt])
    for m_ in range(NTO * 2 // BLK + 1):
        V(lambda m_=m_: nc.vector.tensor_scalar(out=t32, in0=cnt[:], scalar1=float(m_ * BLK + 1), scalar2=None, op0=ALU.is_ge), r=[B_cnt], w=[B_rt])
        V(lambda: nc.vector.tensor_tensor(out=nblk, in0=nblk, in1=t32, op=ALU.add), r=[B_rt], w=[B_rt])
    V(lambda: nc.vector.tensor_tensor_scan(out=incl, data0=ones32, data1=nblk, initial=0.0, op0=ALU.mult, op1=ALU.add), r=[B_rt, B_misc], w=[B_rt])
    V(lambda: nc.vector.tensor_tensor(out=t32, in0=incl, in1=nblk, op=ALU.subtract), r=[B_rt], w=[B_rt])
    V(lambda: nc.vector.tensor_scalar(out=sslot[:], in0=t32, scalar1=float(BLK), scalar2=None, op0=ALU.mult), r=[B_rt], w=[B_ss])
    V(lambda: nc.vector.memset(bacc, 0.0), w=[B_rt])
    for e_ in range(NEXP):
        V(lambda e_=e_: nc.vector.tensor_scalar(out=t128, in0=iotaB, scalar1=incl[:, e_:e_ + 1], scalar2=None, op0=ALU.is_ge), r=[B_rt, B_misc], w=[B_rt])
        V(lambda: nc.vector.tensor_tensor(out=bacc, in0=bacc, in1=t128, op=ALU.add), r=[B_rt], w=[B_rt])
    V(lambda: nc.vector.tensor_scalar(out=bacc, in0=bacc, scalar1=float(NEXP - 1), scalar2=None, op0=ALU.min), r=[B_rt], w=[B_rt])
    for f_ in range(4):
        V(lambda f_=f_: nc.vector.tensor_scalar(out=t128, in0=bacc, scalar1=512.0, scalar2=float(f_ * 128), op0=ALU.mult, op1=ALU.add), r=[B_rt], w=[B_rt])
        V(lambda: nc.vector.tensor_scalar(out=t128, in0=t128, scalar1=iotap, scalar2=None, op0=ALU.add), r=[B_rt, B_misc], w=[B_rt])
        V(lambda f_=f_: nc.vector.tensor_copy(out=widx[:, f_, :], in_=t128), r=[B_rt], w=[B_bexp])
    dst = rt[:, 288:290]
    dst6 = rt[:, 288:294]
    isA = rt[:, 340:342]
    isB = rt[:, 342:344]
    ohx = rt[:, 0:32]
    for j in range(NOWN):
        for kk in range(2):
            V(lambda j=j, kk=kk: nc.vector.tensor_scalar(out=ohx, in0=iota32, scalar1=pe_all[:, j, 2 + kk:3 + kk], scalar2=None, op0=ALU.is_equal),
              r=[B_misc, B_dest[j]], w=[B_rt])
            V(lambda: nc.vector.tensor_tensor(out=ohx, in0=ohx, in1=sslot[:], op=ALU.mult), r=[B_rt, B_ss], w=[B_rt])
            V(lambda kk=kk: nc.vector.tensor_reduce(out=dst[:, kk:kk + 1], in_=ohx, axis=AX.X, op=ALU.add), r=[B_rt], w=[B_rt])
        V(lambda j=j: nc.vector.tensor_tensor(out=dst, in0=dst, in1=pe_all[:, j, 0:2], op=ALU.add), r=[B_rt, B_dest[j]], w=[B_rt])
        V(lambda j=j: nc.vector.tensor_scalar(out=dst, in0=dst, scalar1=float(-TRASH), scalar2=validt[:, j:j + 1], op0=ALU.add, op1=ALU.mult),
          r=[B_rt, B_misc], w=[B_rt])
        V(lambda: nc.vector.tensor_scalar(out=dst, in0=dst, scalar1=float(TRASH), scalar2=None, op0=ALU.add), r=[B_rt], w=[B_rt])
        V(lambda: nc.vector.tensor_scalar(out=isA, in0=dst, scalar1=float(NLh), scalar2=None, op0=ALU.is_lt), r=[B_rt], w=[B_rt])
        V(lambda: nc.vector.tensor_scalar(out=isB, in0=dst, scalar1=float(NL), scalar2=None, op0=ALU.is_lt), r=[B_rt], w=[B_rt])
        V(lambda: nc.vector.tensor_tensor(out=isB, in0=isB, in1=isA, op=ALU.subtract), r=[B_rt], w=[B_rt])
        V(lambda: nc.vector.scalar_tensor_tensor(out=rt[:, 290:292], in0=dst, scalar=float(-NLh), in1=isA, op0=ALU.add, op1=ALU.mult), r=[B_rt], w=[B_rt])
        V(lambda: nc.vector.scalar_tensor_tensor(out=rt[:, 292:294], in0=dst, scalar=float(-NL), in1=isB, op0=ALU.add, op1=ALU.mult), r=[B_rt], w=[B_rt])
        V(lambda: nc.vector.tensor_scalar(out=rt[:, 290:294], in0=rt[:, 290:294], scalar1=float(NLh), scalar2=None, op0=ALU.add), r=[B_rt], w=[B_rt])
        V(lambda j=j: nc.vector.tensor_copy(out=dest_all[:, j, :], in_=dst6), r=[B_rt], w=[B_dest[j]])
        V(lambda j=j: nc.vector.tensor_scalar(out=rt[:, 296:297], in0=iotap, scalar1=float(j * 128), scalar2=None, op0=ALU.add),
          r=[B_misc], w=[B_rt])
        V(lambda: nc.vector.tensor_copy(out=tokid[:], in_=rt[:, 296:297]), r=[B_rt], w=[B_tokid])
        for kk in range(2):
            Dpl(lambda j=j, kk=kk: nc.gpsimd.indirect_dma_start(out=Ls[:, :], out_offset=bass.IndirectOffsetOnAxis(ap=dest_all[:, j, kk:kk + 1], axis=0),
                                                               in_=tokid[:, :], in_offset=None),
                r=[B_dest[j], B_tokid, B_L], w=[B_L])
    S.flush()
    P.release(mk)

    mk = P.mark()
    NSB = BLK // 128
    idxe = [P.sb("idxe%d" % i, [128, NSB], I32) for i in range(2)]
    B_idxe = [Buf("idxe%d" % i) for i in range(2)]
    xg = [P.sb("xg%d" % i, [128, D], BF16) for i in range(3)]
    B_xg = [Buf("xg%d" % i) for i in range(3)]
    xeT = [P.sb("xeT%d" % i, [128, 16, BLK], BF16) for i in range(2)]
    B_xeT = [Buf("xeT%d" % i) for i in range(2)]
    hT = P.sb("hT", [128, 8, BLK], BF16)
    B_hT = Buf("hT")
    wgs = [P.sb("wgs%d" % i, [128, 16, 256], BF16) for i in range(3)]
    wus = [P.sb("wus%d" % i, [128, 16, 256], BF16) for i in range(3)]
    B_wgs = [Buf("wgs%d" % i) for i in range(3)]
    B_wus = [Buf("wus%d" % i) for i in range(3)]
    wds = [P.sb("wds%d" % i, [128, 8, 512], BF16) for i in range(3)]
    B_wds = [Buf("wds%d" % i) for i in range(3)]
    sg = [P.sb("sg%d" % i, [128, 512], F32) for i in range(2)]
    B_sg = [Buf("sg%d" % i) for i in range(2)]
    yb = [P.sb("yb%d" % i, [128, 512], F32) for i in range(3)]
    B_yb = [Buf("yb%d" % i) for i in range(3)]
    ptr = P.ps("ptr4", [128, 2048], BF16)
    B_ptr = Buf("ptr4")
    pG = [P.ps("pG%d" % i, [128, 512], F32) for i in range(2)]
    pU = [P.ps("pU%d" % i, [128, 512], F32) for i in range(2)]
    B_pG = [Buf("pG%d" % i) for i in range(2)]
    B_pU = [Buf("pU%d" % i) for i in range(2)]
    pY = [P.ps("pY%d" % i, [128, 512], F32) for i in range(2)]
    B_pY = [Buf("pY%d" % i) for i in range(2)]
    cq = {"w": 0, "d": 0, "g": 0, "y": 0, "p": 0, "s": 0, "st": 0}
    def dyn_load(dst_tile, src2, b, f_, Bd):
        Dpl(lambda: nc.gpsimd.indirect_dma_start(out=dst_tile[:].rearrange("p k n -> p (k n)"), out_offset=None, in_=src2[:, :],
                                                 in_offset=bass.IndirectOffsetOnAxis(ap=widx[:, f_, b:b + 1], axis=0)),
            r=[B_bexp, B_wexp], w=[Bd])

    for b in range(NBLK):
        ie = b % 2
        xT = xeT[ie]
        BxT = B_xeT[ie]
        Dsp(lambda b=b, ie=ie: nc.sync.dma_start(out=idxe[ie][:], in_=Ls[b * BLK:(b + 1) * BLK, :].rearrange("(s p) o -> p (s o)", p=128),
                                                 allow_slow_non_contiguous=True),
            r=[B_L], w=[B_idxe[ie]])
        for sb_ in range(NSB):
            gi_ = cq["g"] % 3
            cq["g"] += 1
            Dpl(lambda ie=ie, sb_=sb_, gi_=gi_: nc.gpsimd.indirect_dma_start(out=xg[gi_][:, :], out_offset=None, in_=H2s[:, :],
                                                                             in_offset=bass.IndirectOffsetOnAxis(ap=idxe[ie][:, sb_:sb_ + 1], axis=0)),
                r=[B_idxe[ie], B_H2], w=[B_xg[gi_]])
            for jj in range(16):
                T(lambda jj=jj, gi_=gi_: nc.tensor.transpose(out=ptr[:, jj * 128:(jj + 1) * 128], in_=xg[gi_][:, jj * 128:(jj + 1) * 128],
                                                             identity=identb[:]), r=[B_xg[gi_], B_cb], w=[B_ptr])
            A(lambda sb_=sb_, xT=xT: nc.scalar.copy(out=xT[:, 0:8, sb_ * 128:(sb_ + 1) * 128], in_=ptr[:, 0:1024].rearrange("p (k t) -> p k t", k=8)),
              r=[B_ptr], w=[BxT])
            V(lambda sb_=sb_, xT=xT: nc.vector.tensor_copy(out=xT[:, 8:16, sb_ * 128:(sb_ + 1) * 128], in_=ptr[:, 1024:2048].rearrange("p (k t) -> p k t", k=8)),
              r=[B_ptr], w=[BxT])
        for fs in range(4):
            wi = cq["w"] % 3
            cq["w"] += 1
            dyn_load(wgs[wi], WGB, b, fs, B_wgs[wi])
            dyn_load(wus[wi], WUB, b, fs, B_wus[wi])
            for fc in range(2):
                f8 = fs * 2 + fc
                pi = cq["p"] % 2
                cq["p"] += 1
                for k in range(16):
                    T(lambda k=k, fc=fc, wi=wi, pi=pi, xT=xT: nc.tensor.matmul(pG[pi][:, :], lhsT=wgs[wi][:, k, fc * 128:(fc + 1) * 128],
                                                                              rhs=xT[:, k, :], start=(k == 0), stop=(k == 15)),
                      r=[B_wgs[wi], BxT], w=[B_pG[pi]])
                for k in range(16):
                    T(lambda k=k, fc=fc, wi=wi, pi=pi, xT=xT: nc.tensor.matmul(pU[pi][:, :], lhsT=wus[wi][:, k, fc * 128:(fc + 1) * 128],
                                                                              rhs=xT[:, k, :], start=(k == 0), stop=(k == 15)),
                      r=[B_wus[wi], BxT], w=[B_pU[pi]])
                si = cq["s"] % 2
                cq["s"] += 1
                A(lambda pi=pi, si=si: nc.scalar.activation(out=sg[si][:], in_=pG[pi][:, :], func=AF.Silu), r=[B_pG[pi]], w=[B_sg[si]])
                V(lambda pi=pi, si=si, f8=f8: nc.vector.tensor_tensor(out=hT[:, f8, :], in0=sg[si][:], in1=pU[pi][:, :], op=ALU.mult),
                  r=[B_sg[si], B_pU[pi]], w=[B_hT])
        for ns in range(4):
            di = cq["d"] % 3
            cq["d"] += 1
            dyn_load(wds[di], WDB, b, ns, B_wds[di])
            for sb_ in range(NSB):
                pi = cq["y"] % 2
                yi = cq["y"] % 3
                cq["y"] += 1
                for kf in range(8):
                    T(lambda kf=kf, sb_=sb_, di=di, pi=pi: nc.tensor.matmul(pY[pi][:, :], lhsT=hT[:, kf, sb_ * 128:(sb_ + 1) * 128], rhs=wds[di][:, kf, :],
                                                                          start=(kf == 0), stop=(kf == 7)), r=[B_hT, B_wds[di]], w=[B_pY[pi]])
                if yi % 2 == 0:
                    A(lambda pi=pi, yi=yi: nc.scalar.copy(out=yb[yi][:], in_=pY[pi][:, :]), r=[B_pY[pi]], w=[B_yb[yi]])
                else:
                    V(lambda pi=pi, yi=yi: nc.vector.tensor_copy(out=yb[yi][:], in_=pY[pi][:, :]), r=[B_pY[pi]], w=[B_yb[yi]])
                Yh = YsA if b < NBLK // 2 else YsB
                eo = (b % (NBLK // 2)) * BLK + sb_ * 128
                S.dma("act", lambda eo=eo, Yh=Yh, ns=ns, yi=yi: nc.scalar.dma_start(out=Yh[eo:eo + 128, ns * 512:(ns + 1) * 512], in_=yb[yi][:]),
                      [B_yb[yi]], [B_Y])
    S.flush()
    P.release(mk)

    mk = P.mark()
    x5 = [P.sb("x5%d" % i, [128, D], F32) for i in range(2)]
    y0 = [P.sb("y0%d" % i, [128, D], F32) for i in range(2)]
    y1 = [P.sb("y1%d" % i, [128, D], F32) for i in range(2)]
    y2 = [P.sb("y2%d" % i, [128, D], F32) for i in range(2)]
    y3 = [P.sb("y3%d" % i, [128, D], F32) for i in range(2)]
    B_y2 = [Buf("y2%d" % i) for i in range(2)]
    B_y3 = [Buf("y3%d" % i) for i in range(2)]
    B_x5 = [Buf("x5%d" % i) for i in range(2)]
    B_y0 = [Buf("y0%d" % i) for i in range(2)]
    B_y1 = [Buf("y1%d" % i) for i in range(2)]
    gfb = P.sb("gfb", [128, D], F32)
    B_gf = Buf("gfb")
    junk = P.sb("junk5", [128, D], BF16)
    B_junk = Buf("junk5")
    sm = P.sb("sm5", [128, 8], F32)
    B_sm = Buf("sm5")
    Dsp(lambda: nc.sync.dma_start(out=gfb[:], in_=bcast_row(g_fin_d, D)), w=[B_gf])
    for j in range(NOWN):
        i = j % 2
        Dsp(lambda j=j, i=i: nc.sync.dma_start(out=x5[i][:], in_=X1s[j * 128:(j + 1) * 128, :]), r=[B_X1[j]], w=[B_x5[i]])
        for (yt_, By_, Yh, col) in ((y0, B_y0, YsA, 2), (y1, B_y1, YsA, 3), (y2, B_y2, YsB, 4), (y3, B_y3, YsB, 5)):
            Dpl(lambda j=j, i=i, yt_=yt_, Yh=Yh, col=col: nc.gpsimd.indirect_dma_start(
                out=yt_[i][:, :], out_offset=None, in_=Yh[:, :],
                in_offset=bass.IndirectOffsetOnAxis(ap=dest_all[:, j, col:col + 1], axis=0)),
                r=[B_dest[j], B_Y], w=[By_[i]])
        V(lambda i=i: nc.vector.tensor_tensor(out=y0[i][:], in0=y0[i][:], in1=y2[i][:], op=ALU.add), r=[B_y0[i], B_y2[i]], w=[B_y0[i]])
        V(lambda i=i: nc.vector.tensor_tensor(out=y1[i][:], in0=y1[i][:], in1=y3[i][:], op=ALU.add), r=[B_y1[i], B_y3[i]], w=[B_y1[i]])
        V(lambda j=j, i=i: nc.vector.scalar_tensor_tensor(out=x5[i][:], in0=y0[i][:], scalar=w_all[:, j, 0:1], in1=x5[i][:],
                                                          op0=ALU.mult, op1=ALU.add), r=[B_y0[i], B_dest[j], B_x5[i]], w=[B_x5[i]])
        V(lambda j=j, i=i: nc.vector.scalar_tensor_tensor(out=x5[i][:], in0=y1[i][:], scalar=w_all[:, j, 1:2], in1=x5[i][:],
                                                          op0=ALU.mult, op1=ALU.add), r=[B_y1[i], B_dest[j], B_x5[i]], w=[B_x5[i]])
        G(lambda: nc.gpsimd.memset(sm[:, 0:1], 0.0), w=[B_sm])
        A(lambda i=i: nc.scalar.activation(out=junk[:], in_=x5[i][:], func=AF.Square, accum_out=sm[:, 0:1]), r=[B_x5[i], B_sm], w=[B_junk, B_sm])
        A(lambda: nc.scalar.activation(out=sm[:, 1:2], in_=sm[:, 0:1], func=AF.Sqrt, bias=epsb[:, 0:1], scale=1.0 / D), r=[B_sm, B_misc], w=[B_sm])
        V(lambda: nc.vector.reciprocal(out=sm[:, 1:2], in_=sm[:, 1:2]), r=[B_sm], w=[B_sm])
        V(lambda i=i: nc.vector.scalar_tensor_tensor(out=y0[i][:], in0=x5[i][:], scalar=sm[:, 1:2], in1=gfb[:], op0=ALU.mult, op1=ALU.mult),
          r=[B_x5[i], B_sm, B_gf], w=[B_y0[i]])
        Dsp(lambda j=j, i=i: nc.sync.dma_start(out=y_d[j * 128:(j + 1) * 128, :], in_=y0[i][:]), r=[B_y0[i]])
    S.flush()
    P.release(mk)
    return nc, S


def host_consts():
    c = np.zeros((128, 1024), np.float32)
    i = np.arange(128)
    c[:, 0:128] = np.eye(128)
    s, t = np.meshgrid(i, i, indexing="ij")
    c[:, 128:256] = (s <= t)
    c[:, 256:384] = np.where(t <= s, 0.0, NEG)
    c[:, 384:512] = np.where(s <= t, 0.0, NEG)
    c[:, 512:640] = (s >= t)
    c[:, 640:768] = np.where(t >= s, 0.0, NEG)
    c[:, 768:896] = np.where(s >= t, 0.0, NEG)
    c[:, 896:1024] = (s < t)
    sel = np.zeros((8, 8 * 128 + 128 + 8), np.float32)
    for h in range(8):
        sel[h, h * 128:(h + 1) * 128] = 1.0
    sel[:, 1024:1152] = 1.0
    sel[:, 1152:1160] = np.eye(8)
    return c, sel


def host_rcst(CAP, NTO):
    r = np.zeros((128, 240), np.float32)
    r[:, 0:8] = 1.0
    r[:, 40] = np.arange(128)
    r[:, 41] = 1.0
    r[:, 48:80] = np.arange(32)[None, :]
    r[:, 80:208] = np.arange(128)[None, :]
    r[:, 208:240] = 1.0
    nblk = (NTO * 2) // 512 + NEXP
    nblk += nblk % 2
    NL = nblk * 512
    padi = np.full((128, NL // 128 + 1), NTO, np.int32)
    return r, padi


def shared_inputs(inp):
    f = lambda a: np.ascontiguousarray(np.asarray(a, dtype=np.float32))
    cst, sel = host_consts()
    conv = f(inp["mlstm_conv_w"])[0]
    return {
        "w_in": f(inp["w_in"])[0],
        "g_mix": f(inp["norm_mix_g"]).reshape(1, D),
        "g_ffn": f(inp["norm_ffn_g"]).reshape(1, D),
        "g_fin": f(inp["norm_final_g"]).reshape(1, D),
        "b_cg": f(inp["b_cell_gates"]).reshape(1, 32),
        "ln_g": f(inp["gmlp_ln_g"]).reshape(1, D),
        "wsT": np.ascontiguousarray(f(inp["gmlp_w_s"])[0].transpose(2, 0, 1)),
        "bsT": np.ascontiguousarray(f(inp["gmlp_b_s"])[0].T),
        "convw": np.ascontiguousarray(conv.reshape(5, 32, 128).transpose(2, 1, 0)),
        "headg": f(inp["mlstm_head_g"]).reshape(1, D),
        "w_out": f(inp["w_out"])[0],
        "w_r": np.ascontiguousarray(np.concatenate([f(inp["w_router_group"])[0], f(inp["w_router_expert"])[0]], axis=1)),
        "b_r": np.concatenate([f(inp["b_router_group"]).reshape(1, 4), f(inp["b_router_expert"]).reshape(1, 32)], axis=1),
        "w_gate": np.ascontiguousarray(f(inp["w_exp_gate"])[0].reshape(NEXP, 16, 128, 4, 256).transpose(0, 3, 2, 1, 4)).reshape(NEXP * 4 * 128, 16 * 256),
        "w_up": np.ascontiguousarray(f(inp["w_exp_up"])[0].reshape(NEXP, 16, 128, 4, 256).transpose(0, 3, 2, 1, 4)).reshape(NEXP * 4 * 128, 16 * 256),
        "w_down": np.ascontiguousarray(f(inp["w_exp_down"])[0].reshape(NEXP, 8, 128, 4, 512).transpose(0, 3, 2, 1, 4)).reshape(NEXP * 4 * 128, 8 * 512),
        "cst": cst,
        "sel": sel,
    }


NOWN_FULL = 64
NCTX_FULL = 64
CAP_FULL = 768


def flip_shared(sh):
    f = dict(sh)
    w = sh["w_in"].copy()
    w[:, 12288:12304] = sh["w_in"][:, 12304:12320]
    w[:, 12304:12320] = sh["w_in"][:, 12288:12304]
    f["w_in"] = w
    b = sh["b_cg"].copy()
    b[:, 0:16] = sh["b_cg"][:, 16:32]
    b[:, 16:32] = sh["b_cg"][:, 0:16]
    f["b_cg"] = b
    f["wsT"] = np.ascontiguousarray(sh["wsT"][::-1, :, ::-1])
    f["bsT"] = np.ascontiguousarray(sh["bsT"][::-1])
    f["convw"] = np.ascontiguousarray(sh["convw"][:, :, ::-1])
    return f


def kernel(**inp):
    xp = np.asarray(inp["x_prompt"], dtype=np.float32)
    xs = np.asarray(inp["x_sample"], dtype=np.float32)
    NTO = NOWN_FULL * 128
    NT = (NOWN_FULL + NCTX_FULL) * 128
    sh = shared_inputs(inp)
    sh["rcst"], sh["padi"] = host_rcst(CAP_FULL, NTO)
    shf = flip_shared(sh)
    nc, S = build(NOWN_FULL, NCTX_FULL, CAP_FULL)
    in_maps = []
    ones_v = np.ones((128, NOWN_FULL), np.float32)
    for c in range(8):
        m = dict(shf if c == 5 else sh)
        x = np.zeros((NT, D), np.float32)
        valid = np.zeros((128, NOWN_FULL), np.float32)
        keep = np.zeros((128, 1), np.float32)
        if c < 4:
            x[:NTO] = xp[c]
            valid = ones_v
        elif c == 4:
            x[:] = xs[0]
            valid = ones_v
            keep[:] = 1.0
        elif c == 5:
            x[:] = xs[0][::-1]
            valid = ones_v
            keep[:] = 1.0
        m.update({"x": x, "valid": valid, "keep": keep})
        in_maps.append(m)
    res = run_bass_kernel_spmd(nc, in_maps, core_ids=list(range(8)))
    yp = np.stack([res.results[c]["y"] for c in range(4)], axis=0).astype(np.float32)
    ys = np.concatenate([res.results[4]["y"], res.results[5]["y"][::-1]], axis=0)[None].astype(np.float32)
    return (yp, ys)
```

```python
import numpy as np
import concourse.bass as bass
import concourse.mybir as mybir
from concourse.bass_utils import run_bass_kernel_spmd

F32 = mybir.dt.float32
BF16 = mybir.dt.bfloat16
I32 = mybir.dt.int32
AF = mybir.ActivationFunctionType
ALU = mybir.AluOpType
AX = mybir.AxisListType

D = 2048
DIN = 16416
NEXP = 32
DEXP = 1024
EPS = 1e-6
NEG = -30000.0
SEM_CHUNK = 20000


class Buf:
    __slots__ = ("name", "lw", "lr")

    def __init__(self, name):
        self.name = name
        self.lw = {}
        self.lr = {}


class Sched:
    def __init__(self, nc, n_dma_slots=8):
        self.nc = nc
        self.eng = {"pe": nc.tensor, "act": nc.scalar, "dve": nc.vector, "pool": nc.gpsimd, "sp": nc.sync}
        self.ops = []
        self.n_dma_slots = n_dma_slots
        self.dma_rr = {"sp": 0, "act": 0, "pool": 0}
        self.base = 0
        self.floor = 0
        self.seen = {e: {} for e in self.eng}
        self.last_on_slot = {}
        self.cnt = {}
        self.sems = {}
        self.n_waits = 0
        self.n_ops = 0

    def op(self, eng, fn, reads=(), writes=()):
        self.ops.append((eng, fn, tuple(reads), tuple(writes), None))

    def dma(self, q, fn, reads=(), writes=(), group=None, nslots=None):
        key = q if group is None else (q, group)
        n = self.n_dma_slots if nslots is None else nslots
        i = self.dma_rr.get(key, 0)
        self.dma_rr[key] = (i + 1) % n
        slot = i if group is None else "%s%d" % (group, i)
        self.ops.append((q, fn, tuple(reads), tuple(writes), slot))

    def _sem_for(self, t, k):
        step = 1 if not isinstance(t, tuple) else 16
        ch = SEM_CHUNK if step == 1 else SEM_CHUNK // 16
        c = (k - 1) // ch
        key = (t, c)
        if key not in self.sems:
            nm = ("s_%s_%d" % (t, c)) if not isinstance(t, tuple) else ("d_%s%s_%d" % (t[1], t[2], c))
            self.sems[key] = self.nc.semaphore(nm).__enter__()
        return self.sems[key], ((k - 1) % ch + 1) * step

    def flush(self):
        ops = self.ops
        n = len(ops)
        base = self.base
        track_of = [None] * n
        deps = [None] * n
        floor = self.floor
        for li, (e, fn, reads, writes, slot) in enumerate(ops):
            i = base + li
            tr = e if slot is None else ("dma", e, slot)
            track_of[li] = tr
            d = {}
            same = (slot is None)

            def need(t, j):
                if j is None or j < floor:
                    return
                if same and t == e and (e == "pe" or e == "sp"):
                    return
                if d.get(t, -1) < j:
                    d[t] = j

            for b in reads:
                for t, j in b.lw.items():
                    need(t, j)
            for b in writes:
                for t, j in b.lw.items():
                    need(t, j)
                for t, j in b.lr.items():
                    if same and t == e:
                        continue
                    need(t, j)
            if slot is not None:
                need(tr, self.last_on_slot.get(tr))
                self.last_on_slot[tr] = i
            sd = self.seen[e]
            fd = {}
            for t, j in d.items():
                if sd.get(t, -1) < j:
                    sd[t] = j
                    fd[t] = j
            deps[li] = fd
            for b in reads:
                b.lr[tr] = i
            for b in writes:
                b.lw[tr] = i
        milestone = [False] * n
        for li in range(n):
            for t, j in deps[li].items():
                milestone[j - base] = True
            if ops[li][4] is not None:
                milestone[li] = True
        last_of_track = {}
        for li in range(n):
            last_of_track[track_of[li]] = li
        for t, lj in last_of_track.items():
            milestone[lj] = True
        ev = [None] * n
        for li in range(n):
            if milestone[li]:
                t = track_of[li]
                self.cnt[t] = self.cnt.get(t, 0) + 1
                ev[li] = self.cnt[t]
        for li, (e, fn, reads, writes, slot) in enumerate(ops):
            eng = self.eng[e]
            for t, j in deps[li].items():
                s, v = self._sem_for(t, ev[j - base])
                eng.wait_ge(s, v)
                self.n_waits += 1
            ins = fn()
            if milestone[li]:
                s, v = self._sem_for(track_of[li], ev[li])
                ins.then_inc(s, 16 if slot is not None else 1)
        for t, lj in last_of_track.items():
            s, v = self._sem_for(t, ev[lj])
            for en, eng in self.eng.items():
                if en == t:
                    continue
                eng.wait_ge(s, v)
        self.n_ops += n
        self.base = base + n
        self.floor = self.base
        self.ops = []


class Pools:
    def __init__(self, nc):
        self.nc = nc
        self.stack = []

    def sb(self, name, shape, dt):
        cm = self.nc.sbuf_tensor("sb_" + name, shape, dt)
        h = cm.__enter__()
        self.stack.append(cm)
        return h

    def ps(self, name, shape, dt):
        cm = self.nc.psum_tensor("ps_" + name, shape, dt)
        h = cm.__enter__()
        self.stack.append(cm)
        return h

    def mark(self):
        return len(self.stack)

    def release(self, mark):
        while len(self.stack) > mark:
            self.stack.pop().__exit__(None, None, None)


def build(NOWN, NCTX, CAP, debug=False):
    assert NOWN % 4 == 0 and NCTX % 4 == 0 and CAP % 128 == 0
    NCH = NOWN + NCTX
    NT = NCH * 128
    NTO = NOWN * 128
    NTILE = NCH // 4
    NTILE_OWN = NOWN // 4
    BLK = 512
    NBLK = (NTO * 2) // BLK + NEXP
    if NBLK % 2:
        NBLK += 1
    NL = NBLK * BLK
    TRASH = NL
    nc = bass.Bass("TRN2", target_bir_lowering=False)
    S = Sched(nc)
    P = Pools(nc)
    skind = "ExternalOutput" if debug else "Internal"

    def din(name, shape, dt=F32):
        return nc.dram_tensor(name, list(shape), dt, kind="ExternalInput").ap()

    def dscr(name, shape, dt):
        return nc.dram_tensor(name, list(shape), dt, kind=skind).ap()

    x_d = din("x", [NT, D])
    valid_d = din("valid", [128, NOWN])
    keep_d = din("keep", [128, 1])
    w_in_d = din("w_in", [D, DIN])
    g_mix_d = din("g_mix", [1, D])
    g_ffn_d = din("g_ffn", [1, D])
    g_fin_d = din("g_fin", [1, D])
    b_cg_d = din("b_cg", [1, 32])
    ln_g_d = din("ln_g", [1, D])
    wsT_d = din("wsT", [128, 8, 128])
    bsT_d = din("bsT", [128, 8])
    convw_d = din("convw", [128, 32, 5])
    headg_d = din("headg", [1, D])
    w_out_d = din("w_out", [D, D])
    w_r_d = din("w_r", [D, 36])
    b_r_d = din("b_r", [1, 36])
    wg_d = din("w_gate", [NEXP * 4 * 128, 16 * 256])
    wu_d = din("w_up", [NEXP * 4 * 128, 16 * 256])
    wd_d = din("w_down", [NEXP * 4 * 128, 8 * 512])
    cst_d = din("cst", [128, 1024])
    sel_d = din("sel", [8, 8 * 128 + 128 + 8])
    rcst_d = din("rcst", [128, 240])
    padi_d = din("padi", [128, NL // 128 + 1], I32)
    y_d = nc.dram_tensor("y", [NTO, D], F32, kind="ExternalOutput").ap()

    SL_C0 = ([2048 + 512 * i for i in range(4)] + [512 * i for i in range(4)] + [12320 + 512 * i for i in range(4)]
             + [10240 + 512 * i for i in range(4)] + [14368 + 512 * i for i in range(4)] + [8192 + 512 * i for i in range(4)]
             + [12288] + [4096 + 512 * i for i in range(8)])
    SID = {c0: i for i, c0 in enumerate(SL_C0)}
    winb = dscr("winb", [len(SL_C0), 128, 16, 512], BF16)
    QT = dscr("QT", [NCH, 128, 16, 128], BF16)
    KT = dscr("KT", [NCH, 128, 16, 128], BF16)
    Vs = dscr("Vs", [NT, D], BF16)
    A1s = dscr("A1s", [NT, D], BF16)
    G2s = dscr("G2s", [NT, D], BF16)
    GAT = dscr("GAT", [NT, 32], F32)
    HFs = dscr("HFs", [NT, D], F32)
    X1s = dscr("X1s", [NT, D], F32)
    H2s = dscr("H2s", [NTO + 128, D], BF16)
    Ls = dscr("Ls", [NL + 128, 1], I32)
    NLh = NL // 2
    YsA = dscr("YsA", [NLh + 128, D], F32)
    YsB = dscr("YsB", [NLh + 128, D], F32)
    WGB = dscr("WGB", [NEXP * 4 * 128, 16 * 256], BF16)
    WUB = dscr("WUB", [NEXP * 4 * 128, 16 * 256], BF16)
    WDB = dscr("WDB", [NEXP * 4 * 128, 8 * 512], BF16)
    B_wexp = Buf("wexp")
    cast_jobs = []
    for r_ in range(NEXP * 4):
        for (dst_, src_) in ((WGB, wg_d), (WUB, wu_d), (WDB, wd_d)):
            cast_jobs.append((dst_, src_, r_))
    cast_pos = [0]

    def issue_casts(n_):
        for (dst_, src_, r_) in cast_jobs[cast_pos[0]:cast_pos[0] + n_]:
            S.dma("pool", lambda dst_=dst_, src_=src_, r_=r_: nc.gpsimd.dma_start(out=dst_[r_ * 128:(r_ + 1) * 128, :],
                                                                                 in_=src_[r_ * 128:(r_ + 1) * 128, :]),
                  (), [B_wexp], group="cast", nslots=6)
        cast_pos[0] += n_
    n_iter_mlstm = NOWN + NCH
    casts_per_iter = (len(cast_jobs) + n_iter_mlstm - 1) // n_iter_mlstm
    B_winb = [Buf("winb%d" % i) for i in range(64)]
    B_QT = [Buf("QT%d" % j) for j in range(NCH)]
    B_KT = [Buf("KT%d" % j) for j in range(NCH)]
    B_V = [Buf("V%d" % j) for j in range(NCH)]
    B_A1 = [Buf("A1%d" % j) for j in range(NCH)]
    B_G2 = [Buf("G2%d" % j) for j in range(NCH)]
    B_GAT = [Buf("GAT%d" % j) for j in range(NCH)]
    B_HF = [Buf("HF%d" % j) for j in range(NCH)]
    B_X1 = [Buf("X1%d" % j) for j in range(NCH)]
    B_H2 = Buf("H2")
    B_L = Buf("L")
    B_Y = Buf("Y")

    cst = P.sb("cst", [128, 1024], F32)
    B_cst = Buf("cst")
    identf = cst[:, 0:128]
    T_f, MA_f, MAT_f = cst[:, 128:256], cst[:, 256:384], cst[:, 384:512]
    T_b, MA_b, MAT_b = cst[:, 512:640], cst[:, 640:768], cst[:, 768:896]
    trist = cst[:, 896:1024]
    selt = P.sb("selt", [8, 8 * 128 + 128 + 8], F32)
    B_sel = Buf("sel")
    ones8 = selt[:, 1024:1152]
    ident8 = selt[:, 1152:1160]
    identb = P.sb("identb", [128, 128], BF16)
    tristb = P.sb("tristb", [128, 128], BF16)
    onesb = P.sb("onesb", [128, 128], BF16)
    B_cb = Buf("constb")
    epsb = P.sb("epsb", [128, 1], F32)
    validt = P.sb("validt", [128, NOWN], F32)
    keept = P.sb("keept", [128, 1], F32)
    rcst = P.sb("rcst", [128, 240], F32)
    iota32 = rcst[:, 48:80]
    iotaB = rcst[:, 80:208]
    ones32 = rcst[:, 208:240]
    pe_all = P.sb("pe_all", [128, NOWN, 4], F32)
    widx = P.sb("widx", [128, 4, 128], I32)
    sslot = P.sb("sslot", [128, 32], F32)
    B_bexp = Buf("bexp")
    B_ss = Buf("sslot")
    iotap = rcst[:, 40:41]
    onec = rcst[:, 41:42]
    dest_all = P.sb("dest_all", [128, NOWN, 6], I32)
    w_all = P.sb("w_all", [128, NOWN, 2], F32)
    B_dest = [Buf("dest%d" % j) for j in range(NCH)]
    B_misc = Buf("misc")

    def V(fn, r=(), w=()):
        S.op("dve", fn, r, w)

    def A(fn, r=(), w=()):
        S.op("act", fn, r, w)

    def G(fn, r=(), w=()):
        S.op("pool", fn, r, w)

    def T(fn, r=(), w=()):
        S.op("pe", fn, r, w)

    def Dsp(fn, r=(), w=()):
        S.dma("sp", fn, r, w)

    def Dpl(fn, r=(), w=()):
        S.dma("pool", fn, r, w)

    def bcast_row(dram_row, n):
        return dram_row.partition_broadcast(128)

    Dsp(lambda: nc.sync.dma_start(out=cst[:], in_=cst_d), w=[B_cst])
    Dsp(lambda: nc.sync.dma_start(out=selt[:], in_=sel_d), w=[B_sel])
    Dsp(lambda: nc.sync.dma_start(out=validt[:], in_=valid_d), w=[B_misc])
    Dsp(lambda: nc.sync.dma_start(out=keept[:], in_=keep_d), w=[B_misc])
    V(lambda: nc.vector.tensor_copy(out=identb[:], in_=identf), r=[B_cst], w=[B_cb])
    V(lambda: nc.vector.tensor_copy(out=tristb[:], in_=trist), r=[B_cst], w=[B_cb])
    G(lambda: nc.gpsimd.memset(onesb[:], 1.0), w=[B_cb])
    G(lambda: nc.gpsimd.memset(epsb[:], EPS), w=[B_misc])
    Dsp(lambda: nc.sync.dma_start(out=rcst[:], in_=rcst_d), w=[B_misc])
    for sid_, c0_ in enumerate(SL_C0):
        ncl_ = 32 if c0_ == 12288 else 512
        Dpl(lambda sid_=sid_, c0_=c0_, ncl_=ncl_: nc.gpsimd.dma_start(
            out=winb[sid_][:, :, 0:ncl_], in_=w_in_d[:, c0_:c0_ + ncl_].rearrange("(k p) n -> p k n", p=128)), w=[B_winb[sid_]])

    mk = P.mark()
    xin = [P.sb("xin%d" % i, [128, D], F32) for i in range(2)]
    B_xin = [Buf("xin%d" % i) for i in range(2)]
    xnb = P.sb("xnb", [128, D], BF16)
    B_xnb = Buf("xnb")
    junk = xnb
    B_junk = B_xnb
    xnT = P.sb("xnT", [128, 16, 512], BF16)
    B_xnT = Buf("xnT")
    gbc = P.sb("gbc", [128, D], F32)
    lngb = P.sb("lngb", [128, D], F32)
    B_gb = Buf("gbc")
    wsl = [P.sb("wsl%d" % i, [128, 16, 512], BF16) for i in range(3)]
    B_wsl = [Buf("wsl%d" % i) for i in range(3)]
    big = P.sb("big", [128, 4, D], F32)
    B_big = [Buf("big%d" % c) for c in range(4)]
    vn = P.sb("vn", [128, 4, D], BF16)
    B_vn = [Buf("vn%d" % c) for c in range(4)]
    qkb = P.sb("qkb", [128, 16, 512], BF16)
    B_qkb = Buf("qkb")
    qlast = P.sb("qlast", [128, 2, 16, 128], BF16)
    B_qlast = [Buf("qlast0"), Buf("qlast1")]
    tmpA = [P.sb("tmpA%d" % i, [128, 512], F32) for i in range(2)]
    B_tmpA = [Buf("tmpA%d" % i) for i in range(2)]
    tmpB = [P.sb("tmpB%d" % i, [128, 512], BF16) for i in range(3)]
    B_tmpB = [Buf("tmpB%d" % i) for i in range(3)]
    cw = [P.sb("cw%d" % i, [128, 516], F32) for i in range(2)]
    B_cw = [Buf("cw%d" % i) for i in range(2)]
    acc = [P.sb("acc%d" % i, [128, 512], F32) for i in range(2)]
    B_acc = [Buf("acc%d" % i) for i in range(2)]
    carry = P.sb("carry", [128, 32, 4], F32)
    B_carry = Buf("carry")
    convw = P.sb("convw", [128, 32, 5], F32)
    wsTb = P.sb("wsTb", [128, 8, 128], BF16)
    bsT = P.sb("bsT", [128, 8], F32)
    bcgb = P.sb("bcgb", [128, 32], F32)
    B_c1 = Buf("c1")
    sm = P.sb("sm", [128, 64], F32)
    B_sm = Buf("sm")
    st6 = P.sb("st6", [128, 4, 6], F32)
    B_st6 = Buf("st6")
    gt = P.sb("gt", [128, 4, 32], F32)
    B_gt = Buf("gt")
    pa = [P.ps("pa%d" % i, [128, 512], F32) for i in range(4)]
    B_pa = [Buf("pa%d" % i) for i in range(4)]
    ptr = P.ps("ptr", [128, 2048], BF16)
    B_ptr = Buf("ptr")
    psp = [P.ps("psp%d" % i, [128, 512], F32) for i in range(2)]
    B_psp = [Buf("psp%d" % i) for i in range(2)]

    Dsp(lambda: nc.sync.dma_start(out=gbc[:], in_=bcast_row(g_mix_d, D)), w=[B_gb])
    Dsp(lambda: nc.sync.dma_start(out=lngb[:], in_=bcast_row(ln_g_d, D)), w=[B_gb])
    Dsp(lambda: nc.sync.dma_start(out=convw[:], in_=convw_d), w=[B_c1])
    Dpl(lambda: nc.gpsimd.dma_start(out=wsTb[:], in_=wsT_d), w=[B_c1])
    Dsp(lambda: nc.sync.dma_start(out=bsT[:], in_=bsT_d), w=[B_c1])
    Dsp(lambda: nc.sync.dma_start(out=bcgb[:], in_=bcast_row(b_cg_d, 32)), w=[B_c1])
    G(lambda: nc.gpsimd.memset(carry[:], 0.0), w=[B_carry])

    st = {"w": 0, "pa": 0, "tA": 0, "tB": 0, "cw": 0}

    def load_slice(c0, ncols):
        i = st["w"] % 3
        st["w"] += 1
        Dsp(lambda: nc.sync.dma_start(out=wsl[i][:, :, 0:ncols], in_=winb[SID[c0]][:, :, 0:ncols]),
            r=[B_winb[SID[c0]]], w=[B_wsl[i]])
        return i

    def rms_rstd(src_ap, ss_col, rs_col, rd):
        G(lambda: nc.gpsimd.memset(ss_col, 0.0), w=[B_sm])
        A(lambda: nc.scalar.activation(out=junk[:], in_=src_ap, func=AF.Square, accum_out=ss_col),
          r=rd + [B_sm], w=[B_junk, B_sm])
        A(lambda: nc.scalar.activation(out=rs_col, in_=ss_col, func=AF.Sqrt, bias=epsb[:, 0:1], scale=1.0 / D),
          r=[B_sm, B_misc], w=[B_sm])
        V(lambda: nc.vector.reciprocal(out=rs_col, in_=rs_col), r=[B_sm], w=[B_sm])

    def transpose16(src, B_src, dst_fn, B_dst, evac_engs=("act", "dve")):
        for j in range(16):
            T(lambda j=j: nc.tensor.transpose(out=ptr[:, j * 128:(j + 1) * 128], in_=src[:, j * 128:(j + 1) * 128],
                                              identity=identb[:]), r=[B_src, B_cb], w=[B_ptr])
        for hlf in range(2):
            src_ps = ptr[:, hlf * 1024:(hlf + 1) * 1024].rearrange("p (k t) -> p k t", k=8)
            if evac_engs[hlf] == "act":
                A(lambda hlf=hlf, src_ps=src_ps: nc.scalar.copy(out=dst_fn(hlf), in_=src_ps), r=[B_ptr], w=[B_dst])
            else:
                V(lambda hlf=hlf, src_ps=src_ps: nc.vector.tensor_copy(out=dst_fn(hlf), in_=src_ps), r=[B_ptr], w=[B_dst])

    def layoutA(c0, ncols, evac):
        wi = load_slice(c0, ncols)
        for c in range(4):
            pi = st["pa"] % 4
            st["pa"] += 1
            for k in range(16):
                T(lambda k=k, c=c, pi=pi: nc.tensor.matmul(pa[pi][:, 0:ncols], lhsT=xnT[:, k, c * 128:(c + 1) * 128],
                                                          rhs=wsl[wi][:, k, 0:ncols], start=(k == 0), stop=(k == 15)),
                  r=[B_xnT, B_wsl[wi]], w=[B_pa[pi]])
            evac(c, pi)

    def write_qk_chunks(which, dst, B_dst, ti, final=False):
        j0 = ti * 4
        if not final:
            if ti > 0:
                G(lambda: nc.gpsimd.tensor_copy(out=qlast[:, which, :, 126:128], in_=qkb[:, :, 0:2]),
                  r=[B_qkb], w=[B_qlast[which]])
                Dpl(lambda: nc.gpsimd.dma_start(out=dst[j0 - 1], in_=qlast[:, which, :, :]), r=[B_qlast[which]], w=[B_dst[j0 - 1]])
            for c in range(3):
                Dpl(lambda c=c: nc.gpsimd.dma_start(out=dst[j0 + c], in_=qkb[:, :, c * 128 + 2:c * 128 + 130]),
                    r=[B_qkb], w=[B_dst[j0 + c]])
            G(lambda: nc.gpsimd.tensor_copy(out=qlast[:, which, :, 0:126], in_=qkb[:, :, 386:512]),
              r=[B_qkb], w=[B_qlast[which]])
        else:
            G(lambda: nc.gpsimd.tensor_copy(out=qlast[:, which, :, 126:128], in_=qkb[:, :, 0:2]),
              r=[B_qkb], w=[B_qlast[which]])
            Dpl(lambda: nc.gpsimd.dma_start(out=dst[NCH - 1], in_=qlast[:, which, :, :]), r=[B_qlast[which]], w=[B_dst[NCH - 1]])

    def conv_chunk(cc, sec, ci, src_ps, B_src, ncol=512):
        wi = st["cw"] % 2
        st["cw"] += 1
        V(lambda: nc.vector.tensor_copy(out=cw[wi][:, 0:4], in_=carry[:, cc, :]), r=[B_carry], w=[B_cw[wi]])
        if src_ps is not None:
            A(lambda: nc.scalar.copy(out=cw[wi][:, 4:4 + ncol], in_=src_ps), r=[B_src], w=[B_cw[wi]])
        else:
            G(lambda: nc.gpsimd.memset(cw[wi][:, 4:4 + ncol], 0.0), w=[B_cw[wi]])
        eng = V
        ev = nc.vector
        A(lambda: nc.scalar.activation(out=acc[wi][:, 0:ncol], in_=cw[wi][:, 0:ncol], func=AF.Copy, scale=convw[:, cc, 0:1]),
          r=[B_cw[wi], B_c1], w=[B_acc[wi]])
        for j in range(1, 5):
            eng(lambda j=j: ev.scalar_tensor_tensor(out=acc[wi][:, 0:ncol], in0=cw[wi][:, j:j + ncol], scalar=convw[:, cc, j:j + 1],
                                                    in1=acc[wi][:, 0:ncol], op0=ALU.mult, op1=ALU.add),
                r=[B_cw[wi], B_c1], w=[B_acc[wi]])
        if ncol == 512:
            V(lambda: nc.vector.tensor_copy(out=carry[:, cc, :], in_=cw[wi][:, 512:516]), r=[B_cw[wi]], w=[B_carry])
        if sec == 0:
            A(lambda: nc.scalar.activation(out=qkb[:, ci, 0:ncol], in_=acc[wi][:, 0:ncol], func=AF.Silu), r=[B_acc[wi]], w=[B_qkb])
        else:
            A(lambda: nc.scalar.activation(out=acc[wi][:, 0:ncol], in_=acc[wi][:, 0:ncol], func=AF.Silu), r=[B_acc[wi]], w=[B_acc[wi]])
            eng(lambda: ev.tensor_scalar(out=qkb[:, ci, 0:ncol], in0=acc[wi][:, 0:ncol], scalar1=1.0 / 16.0, scalar2=None, op0=ALU.mult),
                r=[B_acc[wi]], w=[B_qkb])

    for ti in range(NTILE):
        t0 = ti * 512
        for c in range(4):
            xi = (ti * 4 + c) % 2
            r0 = t0 + c * 128
            Dpl(lambda xi=xi, r0=r0: nc.gpsimd.dma_start(out=xin[xi][:], in_=x_d[r0:r0 + 128, :]), w=[B_xin[xi]])
            rms_rstd(xin[xi][:], sm[:, 0:1], sm[:, 1:2], [B_xin[xi]])
            V(lambda xi=xi: nc.vector.scalar_tensor_tensor(out=xnb[:], in0=xin[xi][:], scalar=sm[:, 1:2], in1=gbc[:],
                                                           op0=ALU.mult, op1=ALU.mult), r=[B_xin[xi], B_sm, B_gb], w=[B_xnb])
            transpose16(xnb, B_xnb, lambda hlf, c=c: xnT[:, hlf * 8:(hlf + 1) * 8, c * 128:(c + 1) * 128], B_xnT)

        is_ctx = ti >= NTILE_OWN
        for s in range(0 if is_ctx else 4):
            def ev_gv(c, pi, s=s):
                A(lambda: nc.scalar.activation(out=big[:, c, s * 512:(s + 1) * 512], in_=pa[pi][:], func=AF.Gelu_apprx_tanh),
                  r=[B_pa[pi]], w=[B_big[c]])
            layoutA(2048 + s * 512, 512, ev_gv)
        for c in range(0 if is_ctx else 4):
            for s in range(4):
                V(lambda c=c, s=s: nc.vector.bn_stats(out=st6[:, s, :], in_=big[:, c, s * 512:(s + 1) * 512]),
                  r=[B_big[c]], w=[B_st6])
            V(lambda: nc.vector.bn_aggr(out=sm[:, 2:4], in_=st6[:]), r=[B_st6], w=[B_sm])
            A(lambda: nc.scalar.activation(out=sm[:, 4:5], in_=sm[:, 3:4], func=AF.Sqrt, bias=epsb[:, 0:1], scale=1.0),
              r=[B_sm, B_misc], w=[B_sm])
            V(lambda: nc.vector.reciprocal(out=sm[:, 4:5], in_=sm[:, 4:5]), r=[B_sm], w=[B_sm])
            V(lambda c=c: nc.vector.tensor_scalar(out=big[:, c, :], in0=big[:, c, :], scalar1=sm[:, 2:3], scalar2=sm[:, 4:5],
                                                  op0=ALU.subtract, op1=ALU.mult), r=[B_big[c], B_sm], w=[B_big[c]])
            V(lambda c=c: nc.vector.tensor_tensor(out=vn[:, c, :], in0=big[:, c, :], in1=lngb[:], op=ALU.mult),
              r=[B_big[c], B_gb], w=[B_vn[c]])
            for g2 in range(4):
                pi = g2 % 2
                for gg in range(2):
                    g = g2 * 2 + gg
                    T(lambda c=c, g=g, gg=gg, pi=pi: nc.tensor.matmul(psp[pi][:, gg * 256:(gg + 1) * 256], lhsT=wsTb[:, g, :],
                                                                      rhs=vn[:, c, g * 256:(g + 1) * 256], start=True, stop=True),
                      r=[B_c1, B_vn[c]], w=[B_psp[pi]])
                for gg in range(2):
                    g = g2 * 2 + gg
                    V(lambda c=c, g=g, gg=gg, pi=pi: nc.vector.tensor_scalar(out=big[:, c, g * 256:(g + 1) * 256],
                                                                             in0=psp[pi][:, gg * 256:(gg + 1) * 256],
                                                                             scalar1=bsT[:, g:g + 1], scalar2=None, op0=ALU.add),
                      r=[B_psp[pi], B_c1], w=[B_big[c]])
        for s in range(0 if is_ctx else 4):
            def ev_gu(c, pi, s=s):
                ta = st["tA"] % 2
                st["tA"] += 1
                A(lambda: nc.scalar.activation(out=tmpA[ta][:], in_=pa[pi][:], func=AF.Gelu_apprx_tanh), r=[B_pa[pi]], w=[B_tmpA[ta]])
                V(lambda: nc.vector.tensor_tensor(out=big[:, c, s * 512:(s + 1) * 512], in0=tmpA[ta][:],
                                                  in1=big[:, c, s * 512:(s + 1) * 512], op=ALU.mult), r=[B_tmpA[ta], B_big[c]], w=[B_big[c]])
            layoutA(0 + s * 512, 512, ev_gu)
        for s in range(0 if is_ctx else 4):
            def ev_ga(c, pi, s=s, t0=t0):
                ta = st["tA"] % 2
                st["tA"] += 1
                tb = st["tB"] % 3
                st["tB"] += 1
                A(lambda: nc.scalar.activation(out=tmpA[ta][:], in_=pa[pi][:], func=AF.Sigmoid), r=[B_pa[pi]], w=[B_tmpA[ta]])
                V(lambda: nc.vector.tensor_tensor(out=tmpB[tb][:], in0=tmpA[ta][:], in1=big[:, c, s * 512:(s + 1) * 512], op=ALU.mult),
                  r=[B_tmpA[ta], B_big[c]], w=[B_tmpB[tb]])
                j = (t0 // 128) + c
                Dpl(lambda: nc.gpsimd.dma_start(out=A1s[j * 128:(j + 1) * 128, s * 512:(s + 1) * 512], in_=tmpB[tb][:]),
                    r=[B_tmpB[tb]], w=[B_A1[j]])
            layoutA(12320 + s * 512, 512, ev_ga)
        for s in range(0 if is_ctx else 4):
            def ev_o(c, pi, s=s):
                A(lambda: nc.scalar.activation(out=big[:, c, s * 512:(s + 1) * 512], in_=pa[pi][:], func=AF.Sigmoid),
                  r=[B_pa[pi]], w=[B_big[c]])
            layoutA(10240 + s * 512, 512, ev_o)
        for s in range(0 if is_ctx else 4):
            def ev_gb(c, pi, s=s, t0=t0):
                ta = st["tA"] % 2
                st["tA"] += 1
                tb = st["tB"] % 3
                st["tB"] += 1
                A(lambda: nc.scalar.activation(out=tmpA[ta][:], in_=pa[pi][:], func=AF.Sigmoid), r=[B_pa[pi]], w=[B_tmpA[ta]])
                V(lambda: nc.vector.tensor_tensor(out=tmpB[tb][:], in0=tmpA[ta][:], in1=big[:, c, s * 512:(s + 1) * 512], op=ALU.mult),
                  r=[B_tmpA[ta], B_big[c]], w=[B_tmpB[tb]])
                j = (t0 // 128) + c
                Dpl(lambda: nc.gpsimd.dma_start(out=G2s[j * 128:(j + 1) * 128, s * 512:(s + 1) * 512], in_=tmpB[tb][:]),
                    r=[B_tmpB[tb]], w=[B_G2[j]])
            layoutA(14368 + s * 512, 512, ev_gb)
        for s in range(4):
            def ev_v(c, pi, s=s, t0=t0):
                tb = st["tB"] % 3
                st["tB"] += 1
                V(lambda: nc.vector.tensor_copy(out=tmpB[tb][:], in_=pa[pi][:]), r=[B_pa[pi]], w=[B_tmpB[tb]])
                j = (t0 // 128) + c
                Dpl(lambda: nc.gpsimd.dma_start(out=Vs[j * 128:(j + 1) * 128, s * 512:(s + 1) * 512], in_=tmpB[tb][:]),
                    r=[B_tmpB[tb]], w=[B_V[j]])
            layoutA(8192 + s * 512, 512, ev_v)
        wi = load_slice(12288, 32)
        for c in range(4):
            pi = st["pa"] % 4
            st["pa"] += 1
            for k in range(16):
                T(lambda k=k, c=c, pi=pi, wi=wi: nc.tensor.matmul(pa[pi][:, 0:32], lhsT=xnT[:, k, c * 128:(c + 1) * 128],
                                                          rhs=wsl[wi][:, k, 0:32], start=(k == 0), stop=(k == 15)),
                  r=[B_xnT, B_wsl[wi]], w=[B_pa[pi]])
            V(lambda c=c, pi=pi: nc.vector.tensor_tensor(out=gt[:, c, :], in0=pa[pi][:, 0:32], in1=bcgb[:], op=ALU.add),
              r=[B_pa[pi], B_c1], w=[B_gt])
        gtf = gt[:].rearrange("p c (a b) -> p c a b", b=8)
        lsg = sm[:, 8:8 + 32].rearrange("p (c b) -> p c b", b=8)
        lsg2 = sm[:, 40:40 + 24]
        for a_ in (1, 3):
            src = gtf[:, :, a_, :]
            A(lambda src=src: nc.scalar.activation(out=lsg, in_=src, func=AF.Abs), r=[B_gt], w=[B_sm])
            A(lambda: nc.scalar.activation(out=lsg, in_=lsg, func=AF.Exp, scale=-1.0), r=[B_sm], w=[B_sm])
            A(lambda: nc.scalar.activation(out=lsg, in_=lsg, func=AF.Ln, bias=onec, scale=1.0), r=[B_sm, B_misc], w=[B_sm])
            V(lambda src=src: nc.vector.scalar_tensor_tensor(out=src, in0=src, scalar=0.0, in1=lsg, op0=ALU.min, op1=ALU.subtract),
              r=[B_gt, B_sm], w=[B_gt])
        for c in range(4):
            j = (t0 // 128) + c
            Dpl(lambda c=c, j=j: nc.gpsimd.dma_start(out=GAT[j * 128:(j + 1) * 128, :], in_=gt[:, c, :]), r=[B_gt], w=[B_GAT[j]])
        for sec in range(2):
            if sec == 0 and ti > NTILE_OWN - (0 if NCTX > 0 else 1):
                continue
            for s in range(4):
                wi = load_slice(4096 + sec * 2048 + s * 512, 512)
                for jj in range(4):
                    ci = s * 4 + jj
                    cc = sec * 16 + ci
                    pi = st["pa"] % 4
                    st["pa"] += 1
                    for k in range(16):
                        T(lambda k=k, jj=jj, pi=pi, wi=wi: nc.tensor.matmul(pa[pi][:, :], lhsT=wsl[wi][:, k, jj * 128:(jj + 1) * 128],
                                                                            rhs=xnT[:, k, :], start=(k == 0), stop=(k == 15)),
                          r=[B_xnT, B_wsl[wi]], w=[B_pa[pi]])
                    conv_chunk(cc, sec, ci, pa[pi][:, :], B_pa[pi])
            write_qk_chunks(sec, QT if sec == 0 else KT, B_QT if sec == 0 else B_KT, ti)
    for sec in range(1 if NCTX > 0 else 0, 2):
        for ci in range(16):
            conv_chunk(sec * 16 + ci, sec, ci, None, None, ncol=2)
        write_qk_chunks(sec, QT if sec == 0 else KT, B_QT if sec == 0 else B_KT, NTILE, final=True)
    S.flush()
    P.release(mk)

    mk = P.mark()
    qT = [P.sb("qT0", [128, 16, 128], BF16)] * 2
    kT = [P.sb("kT%d" % i, [128, 16, 128], BF16) for i in range(2)]
    vext = [P.sb("vext0", [128, 8, 257], BF16)] * 2
    gat = [P.sb("gat%d" % i, [128, 32], F32) for i in range(2)]
    B_qT = [Buf("qT0")] * 2
    B_kT = [Buf("kT%d" % i) for i in range(2)]
    B_vx = [Buf("vx0")] * 2
    B_gat = [Buf("gat%d" % i) for i in range(2)]
    ktok = P.sb("ktok", [128, D], BF16)
    B_ktok = Buf("ktok")
    Cst = P.sb("Cst", [128, 8, 2, 257], F32)
    Cb = P.sb("Cb", [128, 8, 2, 257], BF16)
    B_C = [Buf("C%d" % h) for h in range(8)]
    B_Cb = [Buf("Cb%d" % h) for h in range(8)]
    mrow = P.sb("mrow", [8, 1], F32)
    B_mrow = Buf("mrow")
    rw = P.sb("rw", [8, 640], F32)
    B_rw = Buf("rw")
    dg = P.sb("dg", [8, 24], F32)
    B_dg = Buf("dg")
    cl = P.sb("cl", [128, 128], F32)
    B_cl = Buf("cl")
    B_den = Buf("den")
    Et = [P.sb("Et%d" % i, [128, 128], F32) for i in range(2)]
    B_Et = [Buf("Et%d" % i) for i in range(2)]
    SmT = [P.sb("SmT%d" % i, [128, 128], BF16) for i in range(2)]
    B_SmT = [Buf("SmT%d" % i) for i in range(2)]
    nint = [P.sb("nint%d" % i, [128, 257], F32) for i in range(2)]
    B_nint = [Buf("nint%d" % i) for i in range(2)]
    numt = [P.sb("numt%d" % i, [128, 257], F32) for i in range(2)]
    B_numt = [Buf("numt%d" % i) for i in range(2)]
    kw = [P.sb("kw%d" % i, [128, 256], BF16) for i in range(2)]
    B_kw = [Buf("kw%d" % i) for i in range(2)]
    hraw = P.sb("hraw", [128, 8, 256], F32)
    B_hraw = Buf("hraw")
    hout = P.sb("hout", [128, D], F32)
    B_hout = Buf("hout")
    hfin = P.sb("hfinb", [128, D], F32)
    B_hfin = Buf("hfinb")
    g2in = P.sb("g2in", [128, D], BF16)
    a1in = P.sb("a1in", [128, D], BF16)
    B_ga = Buf("g2a1")
    hgb = P.sb("hgb", [128, D], F32)
    g2bc = P.sb("g2bc", [128, D], F32)
    B_hg = Buf("hgb")
    mixb = P.sb("mixb", [128, D], BF16)
    B_mixb = Buf("mixb")
    mixT = P.sb("mixT", [128, 16, 128], BF16)
    B_mixT = Buf("mixT")
    woutb = P.sb("woutb", [128, 16, D], BF16)
    B_wout = Buf("woutb")
    x3 = P.sb("x3", [128, D], F32)
    B_x3 = Buf("x3")
    h2b = P.sb("h2b", [128, D], BF16)
    B_h2b = Buf("h2b")
    junk = h2b
    B_junk = B_h2b
    wrb = P.sb("wrb", [128, 16, 36], BF16)
    brb = P.sb("brb", [128, 36], F32)
    B_wr = Buf("wrb")
    rt = P.sb("rt", [128, 352], F32)
    B_rt = Buf("rt")
    cnt = P.sb("cnt", [128, 32], F32)
    B_cnt = Buf("cnt")
    Mb = P.sb("Mb", [128, 32], BF16)
    B_Mb = Buf("Mb")
    tokid = P.sb("tokid", [128, 1], I32)
    B_tokid = Buf("tokid")
    zrow = junk
    B_z = Buf("z")
    sm = P.sb("sm3", [128, 64], F32)
    B_sm = Buf("sm3")

    pS = P.ps("pS", [128, 512], F32)
    B_pS = [Buf("pS%d" % i) for i in range(4)]
    pE = P.ps("pE", [128, 512], F32)
    B_pE = [Buf("pE%d" % i) for i in range(4)]
    pN = [P.ps("pN0", [128, 512], F32)] * 2
    B_pN = [Buf("pN0")] * 2
    pQ = [P.ps("pQ0", [128, 512], F32)] * 2
    B_pQ = [Buf("pQ0")] * 2
    ptr = P.ps("ptr3", [128, 2048], BF16)
    B_ptr = Buf("ptr3")
    pX = P.ps("pX", [128, 1024], F32)
    B_pX = Buf("pX")

    Dsp(lambda: nc.sync.dma_start(out=hgb[:], in_=bcast_row(headg_d, D)), w=[B_hg])
    Dsp(lambda: nc.sync.dma_start(out=g2bc[:], in_=bcast_row(g_ffn_d, D)), w=[B_hg])
    Dsp(lambda: nc.sync.dma_start(out=brb[:], in_=bcast_row(b_r_d, 36)), w=[B_wr])
    Dpl(lambda: nc.gpsimd.dma_start(out=wrb[:], in_=w_r_d.rearrange("(k p) n -> p k n", p=128)), w=[B_wr])
    for k4 in range(4):
        Dpl(lambda k4=k4: nc.gpsimd.dma_start(out=woutb[:, k4 * 4:(k4 + 1) * 4, :],
                                              in_=w_out_d[k4 * 512:(k4 + 1) * 512, :].rearrange("(k p) n -> p k n", p=128)),
            w=[B_wout])
    G(lambda: nc.gpsimd.memset(vext[0][:, :, 256:257], 1.0), w=[B_vx[0]])
    G(lambda: nc.gpsimd.memset(cnt[:], 0.0), w=[B_cnt])
    G(lambda: nc.gpsimd.memset(zrow[:], 0.0), w=[B_z, B_junk])
    G(lambda: nc.gpsimd.memset(x3[0:1, :], 0.0), w=[B_x3])
    Dsp(lambda: nc.sync.dma_start(out=YsA[NLh:NLh + 1, :], in_=x3[0:1, :]), r=[B_x3], w=[B_Y])
    Dsp(lambda: nc.sync.dma_start(out=YsB[NLh:NLh + 1, :], in_=x3[0:1, :]), r=[B_x3], w=[B_Y])
    G(lambda: nc.gpsimd.tensor_copy(out=rt[:, 300:340], in_=rcst[:, 0:40]), r=[B_misc], w=[B_rt])
    Dsp(lambda: nc.sync.dma_start(out=H2s[NTO:NTO + 128, :], in_=zrow[:]), r=[B_z, B_junk], w=[B_H2])
    Dsp(lambda: nc.sync.dma_start(out=Ls.rearrange("(p n) o -> p (n o)", p=128), in_=padi_d), w=[B_L])

    def bf_view(ps_tile, lo, n):
        return ps_tile[:, lo:lo + n // 2].bitcast(BF16)

    def mlstm_loads(j, bi, scan_only=False):
        q_, k_, v_, g_ = qT[bi], kT[bi], vext[bi], gat[bi]
        Bq, Bk, Bv, Bg = B_qT[bi], B_kT[bi], B_vx[bi], B_gat[bi]
        Dsp(lambda: nc.sync.dma_start(out=k_[:], in_=KT[j]), r=[B_KT[j]], w=[Bk])
        Dsp(lambda: nc.sync.dma_start(out=g_[:], in_=GAT[j * 128:(j + 1) * 128, :]), r=[B_GAT[j]], w=[Bg])

    def mlstm_loads_qv(j, bi, scan_only=False):
        q_, v_ = qT[bi], vext[bi]
        Bq, Bv = B_qT[bi], B_vx[bi]
        if not scan_only:
            Dsp(lambda: nc.sync.dma_start(out=q_[:], in_=QT[j]), r=[B_QT[j]], w=[Bq])
        Dsp(lambda: nc.sync.dma_start(out=v_[:, :, 0:256], in_=Vs[j * 128:(j + 1) * 128, :].rearrange("p (h e) -> p h e", h=8)),
            r=[B_V[j]], w=[Bv])

    def mlstm_chunk(j, bwd, bi, scan_only=False, filler=None):
        def adv():
            if filler is not None:
                next(filler, None)
        mlstm_loads_qv(j, bi, scan_only)
        Tm, MA, MAT = (T_b, MA_b, MAT_b) if bwd else (T_f, MA_f, MAT_f)
        gi, gf = (16, 24) if bwd else (0, 8)
        last = 0 if bwd else 127
        q_, k_, v_, g_ = qT[bi], kT[bi], vext[bi], gat[bi]
        Bq, Bk, Bv, Bg = B_qT[bi], B_kT[bi], B_vx[bi], B_gat[bi]
        li = g_[:, gi:gi + 8]
        lf = g_[:, gf:gf + 8]
        for c in range(16):
            T(lambda c=c: nc.tensor.transpose(out=ptr[:, c * 128:(c + 1) * 128], in_=k_[:, c, :], identity=identb[:]),
              r=[Bk, B_cb], w=[B_ptr])
        A(lambda: nc.scalar.copy(out=ktok[:, 0:1024], in_=ptr[:, 0:1024]), r=[B_ptr], w=[B_ktok])
        V(lambda: nc.vector.tensor_copy(out=ktok[:, 1024:2048], in_=ptr[:, 1024:2048]), r=[B_ptr], w=[B_ktok])
        T(lambda: nc.tensor.matmul(pX[:, 0:8], lhsT=Tm, rhs=lf, start=True, stop=True), r=[B_cst, Bg], w=[B_pX])
        T(lambda: nc.tensor.matmul(pX[0:8, 64:192], lhsT=lf, rhs=Tm, start=True, stop=True), r=[B_cst, Bg], w=[B_pX])
        T(lambda: nc.tensor.matmul(pX[0:8, 192:320], lhsT=li, rhs=identf, start=True, stop=True), r=[B_cst, Bg], w=[B_pX])
        Frow, urow = rw[:, 0:128], rw[:, 128:256]
        A(lambda: nc.scalar.copy(out=Frow, in_=pX[0:8, 64:192]), r=[B_pX], w=[B_rw])
        V(lambda: nc.vector.tensor_tensor(out=urow, in0=pX[0:8, 192:320], in1=Frow, op=ALU.subtract), r=[B_pX, B_rw], w=[B_rw])
        umax, FLc, t1, mnew, d1, arow, d2 = (rw[:, 512 + i:513 + i] for i in range(7))
        V(lambda: nc.vector.tensor_reduce(out=umax, in_=urow, axis=AX.X, op=ALU.max), r=[B_rw], w=[B_rw])
        V(lambda: nc.vector.tensor_copy(out=FLc, in_=rw[:, last:last + 1]), r=[B_rw], w=[B_rw])
        V(lambda: nc.vector.tensor_tensor(out=t1, in0=mrow[:, 0:1], in1=umax, op=ALU.max), r=[B_rw, B_mrow], w=[B_rw])
        V(lambda: nc.vector.tensor_tensor(out=mnew, in0=FLc, in1=t1, op=ALU.add), r=[B_rw], w=[B_rw])
        V(lambda: nc.vector.tensor_tensor(out=d1, in0=FLc, in1=mrow[:, 0:1], op=ALU.add), r=[B_rw, B_mrow], w=[B_rw])
        V(lambda: nc.vector.tensor_tensor(out=d1, in0=d1, in1=mnew, op=ALU.subtract), r=[B_rw], w=[B_rw])
        A(lambda: nc.scalar.activation(out=arow, in_=d1, func=AF.Exp), r=[B_rw], w=[B_rw])
        V(lambda: nc.vector.tensor_tensor(out=d2, in0=FLc, in1=mnew, op=ALU.subtract), r=[B_rw], w=[B_rw])
        V(lambda: nc.vector.tensor_scalar(out=dg[:, 0:8], in0=ident8, scalar1=d2, scalar2=None, op0=ALU.mult), r=[B_sel, B_rw], w=[B_dg])
        V(lambda: nc.vector.tensor_scalar(out=dg[:, 8:16], in0=ident8, scalar1=mrow[:, 0:1], scalar2=None, op0=ALU.mult),
          r=[B_sel, B_mrow], w=[B_dg])
        V(lambda: nc.vector.tensor_scalar(out=dg[:, 16:24], in0=ident8, scalar1=arow, scalar2=None, op0=ALU.mult), r=[B_sel, B_rw], w=[B_dg])
        T(lambda: nc.tensor.matmul(pX[:, 8:32], lhsT=ones8, rhs=dg[:, :], start=True, stop=True), r=[B_sel, B_dg], w=[B_pX])
        A(lambda: nc.scalar.copy(out=cl[:, 0:32], in_=pX[:, 0:32]), r=[B_pX], w=[B_cl])
        Fc, d2bc, mpbc, abc, ucol, kwsc, mxu, Gc, wint, enm, den8, rden, tmpc = (cl[:, 8 * i:8 * i + 8] for i in range(13))
        V(lambda: nc.vector.tensor_tensor(out=ucol, in0=li, in1=Fc, op=ALU.subtract), r=[Bg, B_cl], w=[B_cl])
        V(lambda: nc.vector.tensor_tensor(out=kwsc, in0=ucol, in1=d2bc, op=ALU.add), r=[B_cl], w=[B_cl])
        A(lambda: nc.scalar.activation(out=kwsc, in_=kwsc, func=AF.Exp), r=[B_cl], w=[B_cl])
        for h in range(0 if scan_only else 8):
            T(lambda h=h: nc.tensor.matmul(pXd(h), lhsT=selt[:, h * 128:(h + 1) * 128],
                                           rhs=urow, start=True, stop=False), r=[B_sel, B_rw, B_cl], w=[B_pX])
            T(lambda h=h: nc.tensor.matmul(pXd(h), lhsT=identf, rhs=MA, start=False, stop=True), r=[B_cst], w=[B_pX])
        if not scan_only:
            V(lambda: nc.vector.tensor_reduce(out=mxu, in_=pX[:, 0:1024].rearrange("p (h s) -> p h s", h=8), axis=AX.X, op=ALU.max),
              r=[B_pX], w=[B_cl])
            V(lambda: nc.vector.tensor_tensor(out=Gc, in0=mpbc, in1=mxu, op=ALU.max), r=[B_cl], w=[B_cl])
            V(lambda: nc.vector.tensor_tensor(out=wint, in0=mpbc, in1=Gc, op=ALU.subtract), r=[B_cl], w=[B_cl])
            A(lambda: nc.scalar.activation(out=wint, in_=wint, func=AF.Exp), r=[B_cl], w=[B_cl])
            V(lambda: nc.vector.tensor_tensor(out=enm, in0=Fc, in1=Gc, op=ALU.add), r=[B_cl], w=[B_cl])
            A(lambda: nc.scalar.activation(out=enm, in_=enm, func=AF.Exp, scale=-1.0), r=[B_cl], w=[B_cl])
            T(lambda: nc.tensor.matmul(pX[0:8, 0:128], lhsT=Gc, rhs=identf, start=True, stop=True), r=[B_cl, B_cst], w=[B_pX])
            nGrow = rw[:, 256:384]
            A(lambda: nc.scalar.activation(out=nGrow, in_=pX[0:8, 0:128], func=AF.Copy, scale=-1.0), r=[B_pX], w=[B_rw])
        adv()
        adv()
        for h in range(8):
            r4 = h % 4
            i2 = h % 2
            psS = pS[:, r4 * 128:(r4 + 1) * 128]
            psE = pE[:, r4 * 128:(r4 + 1) * 128]
            if not scan_only:
                for dc in range(2):
                    T(lambda h=h, dc=dc, psS=psS: nc.tensor.matmul(psS, lhsT=k_[:, 2 * h + dc, :], rhs=q_[:, 2 * h + dc, :],
                                                                   start=(dc == 0), stop=(dc == 1)), r=[Bq, Bk], w=[B_pS[r4]])
                T(lambda h=h, psE=psE: nc.tensor.matmul(psE, lhsT=selt[:, h * 128:(h + 1) * 128], rhs=nGrow, start=True, stop=False),
                  r=[B_sel, B_rw], w=[B_pE[r4]])
                T(lambda psE=psE: nc.tensor.matmul(psE, lhsT=identf, rhs=MAT, start=False, stop=True), r=[B_cst], w=[B_pE[r4]])
                A(lambda h=h, psE=psE, i2=i2: nc.scalar.activation(out=Et[i2][:], in_=psE, func=AF.Exp, bias=ucol[:, h:h + 1], scale=1.0),
                  r=[B_pE[r4], B_cl], w=[B_Et[i2]])
                V(lambda psS=psS, i2=i2: nc.vector.tensor_tensor(out=SmT[i2][:], in0=psS, in1=Et[i2][:], op=ALU.mult),
                  r=[B_pS[r4], B_Et[i2]], w=[B_SmT[i2]])
                T(lambda h=h, i2=i2: nc.tensor.matmul(pN[i2][:, 0:257], lhsT=SmT[i2][:], rhs=v_[:, h, :], start=True, stop=True),
                  r=[B_SmT[i2], Bv], w=[B_pN[i2]])
                for dc in range(2):
                    T(lambda h=h, dc=dc, i2=i2: nc.tensor.matmul(pQ[i2][:, 0:257], lhsT=q_[:, 2 * h + dc, :], rhs=Cb[:, h, dc, :],
                                                                 start=(dc == 0), stop=(dc == 1)), r=[Bq, B_Cb[h]], w=[B_pQ[i2]])
                A(lambda i2=i2: nc.scalar.copy(out=nint[i2][:], in_=pN[i2][:, 0:257]), r=[B_pN[i2]], w=[B_nint[i2]])
                V(lambda h=h, i2=i2: nc.vector.scalar_tensor_tensor(out=hraw[:, h, :], in0=pQ[i2][:, 0:256], scalar=wint[:, h:h + 1],
                                                                    in1=nint[i2][:, 0:256], op0=ALU.mult, op1=ALU.add),
                  r=[B_pQ[i2], B_cl, B_nint[i2]], w=[B_hraw])
                V(lambda h=h, i2=i2: nc.vector.scalar_tensor_tensor(out=den8[:, h:h + 1], in0=pQ[i2][:, 256:257], scalar=wint[:, h:h + 1],
                                                                    in1=nint[i2][:, 256:257], op0=ALU.mult, op1=ALU.add),
                  r=[B_pQ[i2], B_cl, B_nint[i2]], w=[B_den])
            A(lambda h=h, i2=i2: nc.scalar.activation(out=kw[i2][:], in_=ktok[:, h * 256:(h + 1) * 256], func=AF.Copy,
                                                      scale=kwsc[:, h:h + 1]), r=[B_ktok, B_cl], w=[B_kw[i2]])
            cu = (pXc(0), pXc(1), [B_pX], [B_pX])
            if scan_only:
                cu = [cu, (pN[0][:, 0:257], pQ[0][:, 0:257], [B_pN[0]], [B_pQ[0]]),
                      (pS[:, 0:257], pE[:, 0:257], B_pS, B_pE)][h % 3]
            for dc in range(2):
                T(lambda h=h, dc=dc, i2=i2, cu=cu: nc.tensor.matmul(cu[dc], lhsT=kw[i2][:, dc * 128:(dc + 1) * 128], rhs=v_[:, h, :],
                                                                    start=True, stop=True), r=[B_kw[i2], Bv, B_cl, B_rw], w=cu[2 + dc])
            for dc in range(2):
                V(lambda h=h, dc=dc, cu=cu: nc.vector.scalar_tensor_tensor(out=Cst[:, h, dc, :], in0=Cst[:, h, dc, :], scalar=abc[:, h:h + 1],
                                                                           in1=cu[dc], op0=ALU.mult, op1=ALU.add),
                  r=[B_C[h], B_cl] + cu[2 + dc], w=[B_C[h]])
            A(lambda h=h: nc.scalar.copy(out=Cb[:, h, :, :], in_=Cst[:, h, :, :]), r=[B_C[h]], w=[B_Cb[h]])
            adv()
            adv()
        V(lambda: nc.vector.tensor_copy(out=mrow[:, 0:1], in_=mnew), r=[B_rw, B_dg], w=[B_mrow])
        if filler is not None:
            for _ in filler:
                pass
        if scan_only:
            return None
        A(lambda: nc.scalar.activation(out=tmpc, in_=den8, func=AF.Abs), r=[B_cl, B_den], w=[B_cl])
        V(lambda: nc.vector.tensor_tensor(out=tmpc, in0=tmpc, in1=enm, op=ALU.max), r=[B_cl], w=[B_cl])
        V(lambda: nc.vector.reciprocal(out=rden, in_=tmpc), r=[B_cl], w=[B_cl])
        return rden

    def pXd(h):
        return pX[:, h * 128:(h + 1) * 128]

    def pXc(dc):
        return pX[:, dc * 512:dc * 512 + 257]

    def reset_state(full):
        if full:
            G(lambda: nc.gpsimd.memset(Cst[:], 0.0), w=B_C)
            G(lambda: nc.gpsimd.memset(Cb[:], 0.0), w=B_Cb)
            G(lambda: nc.gpsimd.memset(mrow[:], 0.0), w=[B_mrow])
        else:
            G(lambda: nc.gpsimd.tensor_scalar(out=Cst[:], in0=Cst[:], scalar1=keept[:, 0:1], scalar2=None, op0=ALU.mult),
              r=B_C + [B_misc], w=B_C)
            G(lambda: nc.gpsimd.tensor_scalar(out=Cb[:], in0=Cb[:], scalar1=keept[:, 0:1], scalar2=None, op0=ALU.mult),
              r=B_Cb + [B_misc], w=B_Cb)
            G(lambda: nc.gpsimd.tensor_scalar(out=mrow[:], in0=mrow[:], scalar1=keept[0:8, 0:1], scalar2=None, op0=ALU.mult),
              r=[B_mrow, B_misc], w=[B_mrow])

    reset_state(True)
    mlstm_loads(0, 0)
    for n_, j in enumerate(range(NOWN)):
        issue_casts(casts_per_iter)
        if j + 1 < NOWN:
            mlstm_loads(j + 1, (n_ + 1) % 2)
        rden = mlstm_chunk(j, False, n_ % 2)
        for h in range(8):
            if h % 2:
                A(lambda h=h, rden=rden: nc.scalar.activation(out=hout[:, h * 256:(h + 1) * 256], in_=hraw[:, h, :], func=AF.Copy,
                                                              scale=rden[:, h:h + 1]), r=[B_hraw, B_cl], w=[B_hout])
            else:
                V(lambda h=h, rden=rden: nc.vector.tensor_scalar(out=hout[:, h * 256:(h + 1) * 256], in0=hraw[:, h, :], scalar1=rden[:, h:h + 1],
                                                                 scalar2=None, op0=ALU.mult), r=[B_hraw, B_cl], w=[B_hout])
        Dpl(lambda j=j: nc.gpsimd.dma_start(out=HFs[j * 128:(j + 1) * 128, :], in_=hout[:]), r=[B_hout], w=[B_HF[j]])
    S.flush()

    reset_state(True)
    def merge_gen(j):
        Dsp(lambda j=j: nc.sync.dma_start(out=g2in[:], in_=G2s[j * 128:(j + 1) * 128, :]), r=[B_G2[j]], w=[B_ga])
        Dsp(lambda j=j: nc.sync.dma_start(out=a1in[:], in_=A1s[j * 128:(j + 1) * 128, :]), r=[B_A1[j]], w=[B_ga])
        Dsp(lambda j=j: nc.sync.dma_start(out=x3[:], in_=x_d[j * 128:(j + 1) * 128, :]), w=[B_x3])
        G(lambda: nc.gpsimd.memset(sm[:, 0:8], 0.0), w=[B_sm])
        for h in range(8):
            A(lambda h=h: nc.scalar.activation(out=junk[:, 0:256], in_=hout[:, h * 256:(h + 1) * 256], func=AF.Square,
                                               accum_out=sm[:, h:h + 1]), r=[B_hout, B_sm], w=[B_junk, B_sm])
        yield
        A(lambda: nc.scalar.activation(out=sm[:, 8:16], in_=sm[:, 0:8], func=AF.Sqrt, bias=epsb[:, 0:1], scale=1.0 / 256.0),
          r=[B_sm, B_misc], w=[B_sm])
        V(lambda: nc.vector.reciprocal(out=sm[:, 8:16], in_=sm[:, 8:16]), r=[B_sm], w=[B_sm])
        for h in range(8):
            eng, ev = (V, nc.vector)
            eng(lambda h=h, ev=ev: ev.scalar_tensor_tensor(out=hout[:, h * 256:(h + 1) * 256], in0=hout[:, h * 256:(h + 1) * 256],
                                                           scalar=sm[:, 8 + h:9 + h], in1=hgb[:, h * 256:(h + 1) * 256],
                                                           op0=ALU.mult, op1=ALU.mult), r=[B_hout, B_sm, B_hg], w=[B_hout])
        yield
        V(lambda: nc.vector.tensor_tensor(out=hout[:], in0=hout[:], in1=g2in[:], op=ALU.mult), r=[B_hout, B_ga], w=[B_hout])
        V(lambda: nc.vector.tensor_tensor(out=mixb[:], in0=hout[:], in1=a1in[:], op=ALU.add), r=[B_hout, B_ga], w=[B_mixb])
        yield
        for c in range(16):
            T(lambda c=c: nc.tensor.transpose(out=ptr[:, c * 128:(c + 1) * 128], in_=mixb[:, c * 128:(c + 1) * 128], identity=identb[:]),
              r=[B_mixb, B_cb], w=[B_ptr])
        A(lambda: nc.scalar.copy(out=mixT[:, 0:8, :], in_=ptr[:, 0:1024].rearrange("p (k t) -> p k t", k=8)), r=[B_ptr], w=[B_mixT])
        V(lambda: nc.vector.tensor_copy(out=mixT[:, 8:16, :], in_=ptr[:, 1024:2048].rearrange("p (k t) -> p k t", k=8)), r=[B_ptr], w=[B_mixT])
        yield
        obanks = [(pN[0], B_pN[0]), (pQ[0], B_pQ[0]), (pN[0], B_pN[0]), (pQ[0], B_pQ[0])]
        for ns in range(4):
            pb, Bpb = obanks[ns]
            for k in range(16):
                T(lambda k=k, ns=ns, pb=pb: nc.tensor.matmul(pb[:, :], lhsT=mixT[:, k, :], rhs=woutb[:, k, ns * 512:(ns + 1) * 512],
                                                             start=(k == 0), stop=(k == 15)), r=[B_mixT, B_wout], w=[Bpb])
            V(lambda ns=ns, pb=pb: nc.vector.tensor_tensor(out=x3[:, ns * 512:(ns + 1) * 512], in0=x3[:, ns * 512:(ns + 1) * 512],
                                                           in1=pb[:, :], op=ALU.add), r=[B_x3, Bpb], w=[B_x3])
            yield
        Dpl(lambda j=j: nc.gpsimd.dma_start(out=X1s[j * 128:(j + 1) * 128, :], in_=x3[:]), r=[B_x3], w=[B_X1[j]])
        yield
        G(lambda: nc.gpsimd.memset(sm[:, 16:17], 0.0), w=[B_sm])
        A(lambda: nc.scalar.activation(out=junk[:], in_=x3[:], func=AF.Square, accum_out=sm[:, 16:17]), r=[B_x3, B_sm], w=[B_junk, B_sm])
        A(lambda: nc.scalar.activation(out=sm[:, 17:18], in_=sm[:, 16:17], func=AF.Sqrt, bias=epsb[:, 0:1], scale=1.0 / D),
          r=[B_sm, B_misc], w=[B_sm])
        V(lambda: nc.vector.reciprocal(out=sm[:, 17:18], in_=sm[:, 17:18]), r=[B_sm], w=[B_sm])
        V(lambda: nc.vector.scalar_tensor_tensor(out=h2b[:], in0=x3[:], scalar=sm[:, 17:18], in1=g2bc[:], op0=ALU.mult, op1=ALU.mult),
          r=[B_x3, B_sm, B_hg], w=[B_h2b])
        Dpl(lambda j=j: nc.gpsimd.dma_start(out=H2s[j * 128:(j + 1) * 128, :], in_=h2b[:]), r=[B_h2b], w=[B_H2])
        yield
        for c in range(16):
            T(lambda c=c: nc.tensor.transpose(out=ptr[:, c * 128:(c + 1) * 128], in_=h2b[:, c * 128:(c + 1) * 128], identity=identb[:]),
              r=[B_h2b, B_cb], w=[B_ptr])
        A(lambda: nc.scalar.copy(out=mixT[:, 0:8, :], in_=ptr[:, 0:1024].rearrange("p (k t) -> p k t", k=8)), r=[B_ptr], w=[B_mixT])
        V(lambda: nc.vector.tensor_copy(out=mixT[:, 8:16, :], in_=ptr[:, 1024:2048].rearrange("p (k t) -> p k t", k=8)), r=[B_ptr], w=[B_mixT])
        for k in range(16):
            T(lambda k=k: nc.tensor.matmul(pX[:, 0:36], lhsT=mixT[:, k, :], rhs=wrb[:, k, :], start=(k == 0), stop=(k == 15)),
              r=[B_mixT, B_wr], w=[B_pX])
        yield
        lg = rt[:, 0:36]
        V(lambda: nc.vector.tensor_tensor(out=lg, in0=pX[:, 0:36], in1=brb[:], op=ALU.add), r=[B_pX, B_wr], w=[B_rt])
        gmax, gsum, pg, v1, v2, wk1, wk2, dsum = (rt[:, 40 + i:41 + i] for i in range(8))
        ohg = rt[:, 48:52]
        eg = rt[:, 52:56]
        msk = rt[:, 64:96]
        oh1 = rt[:, 96:128]
        oh2 = rt[:, 128:160]
        Mt = rt[:, 160:192]
        pos = rt[:, 192:224]
        tmp32 = rt[:, 224:256]
        ebase = rt[:, 256:288]
        dst = rt[:, 288:290]
        dst6 = rt[:, 288:294]
        isA = rt[:, 340:342]
        isB = rt[:, 342:344]
        V(lambda: nc.vector.tensor_reduce(out=gmax, in_=lg[:, 0:4], axis=AX.X, op=ALU.max), r=[B_rt], w=[B_rt])
        V(lambda: nc.vector.tensor_scalar(out=ohg, in0=lg[:, 0:4], scalar1=gmax, scalar2=None, op0=ALU.is_equal), r=[B_rt], w=[B_rt])
        V(lambda: nc.vector.tensor_scalar(out=eg, in0=lg[:, 0:4], scalar1=gmax, scalar2=None, op0=ALU.subtract), r=[B_rt], w=[B_rt])
        A(lambda: nc.scalar.activation(out=eg, in_=eg, func=AF.Exp), r=[B_rt], w=[B_rt])
        V(lambda: nc.vector.tensor_reduce(out=gsum, in_=eg, axis=AX.X, op=ALU.add), r=[B_rt], w=[B_rt])
        V(lambda: nc.vector.reciprocal(out=pg, in_=gsum), r=[B_rt], w=[B_rt])
        yield
        for g in range(4):
            V(lambda g=g: nc.vector.tensor_scalar(out=msk[:, g * 8:(g + 1) * 8], in0=lg[:, 4 + g * 8:12 + g * 8], scalar1=ohg[:, g:g + 1],
                                                  scalar2=None, op0=ALU.mult), r=[B_rt], w=[B_rt])
            V(lambda g=g: nc.vector.tensor_scalar(out=tmp32[:, g * 8:(g + 1) * 8], in0=rt[:, 300:308],
                                                  scalar1=ohg[:, g:g + 1], scalar2=None, op0=ALU.mult), r=[B_rt], w=[B_rt])
        V(lambda: nc.vector.tensor_scalar(out=tmp32, in0=tmp32, scalar1=-1.0, scalar2=1e4, op0=ALU.add, op1=ALU.mult), r=[B_rt], w=[B_rt])
        V(lambda: nc.vector.tensor_tensor(out=msk, in0=msk, in1=tmp32, op=ALU.add), r=[B_rt], w=[B_rt])
        V(lambda: nc.vector.tensor_reduce(out=v1, in_=msk, axis=AX.X, op=ALU.max), r=[B_rt], w=[B_rt])
        V(lambda: nc.vector.tensor_scalar(out=oh1, in0=msk, scalar1=v1, scalar2=None, op0=ALU.is_equal), r=[B_rt], w=[B_rt])
        V(lambda: nc.vector.scalar_tensor_tensor(out=tmp32, in0=oh1, scalar=-1e4, in1=msk, op0=ALU.mult, op1=ALU.add), r=[B_rt], w=[B_rt])
        V(lambda: nc.vector.tensor_reduce(out=v2, in_=tmp32, axis=AX.X, op=ALU.max), r=[B_rt], w=[B_rt])
        V(lambda: nc.vector.tensor_scalar(out=oh2, in0=tmp32, scalar1=v2, scalar2=None, op0=ALU.is_equal), r=[B_rt], w=[B_rt])
        yield
        V(lambda: nc.vector.tensor_tensor(out=dsum, in0=v1, in1=v2, op=ALU.subtract), r=[B_rt], w=[B_rt])
        A(lambda: nc.scalar.activation(out=dsum, in_=dsum, func=AF.Sigmoid), r=[B_rt], w=[B_rt])
        V(lambda j=j: nc.vector.tensor_tensor(out=w_all[:, j, 0:1], in0=pg, in1=dsum, op=ALU.mult), r=[B_rt], w=[B_dest[j]])
        V(lambda j=j: nc.vector.tensor_tensor(out=w_all[:, j, 1:2], in0=pg, in1=w_all[:, j, 0:1], op=ALU.subtract), r=[B_rt, B_dest[j]], w=[B_dest[j]])
        yield
        V(lambda j=j: nc.vector.tensor_scalar(out=oh1, in0=oh1, scalar1=validt[:, j:j + 1], scalar2=None, op0=ALU.mult), r=[B_rt, B_misc], w=[B_rt])
        V(lambda j=j: nc.vector.tensor_scalar(out=oh2, in0=oh2, scalar1=validt[:, j:j + 1], scalar2=None, op0=ALU.mult), r=[B_rt, B_misc], w=[B_rt])
        V(lambda: nc.vector.tensor_tensor(out=Mt, in0=oh1, in1=oh2, op=ALU.add), r=[B_rt], w=[B_rt])
        V(lambda: nc.vector.tensor_copy(out=Mb[:], in_=Mt), r=[B_rt], w=[B_Mb])
        T(lambda: nc.tensor.matmul(pX[:, 64:96], lhsT=tristb[:], rhs=Mb[:], start=True, stop=True), r=[B_cb, B_Mb], w=[B_pX])
        T(lambda: nc.tensor.matmul(pX[:, 96:128], lhsT=onesb[:], rhs=Mb[:], start=True, stop=True), r=[B_cb, B_Mb], w=[B_pX])
        V(lambda: nc.vector.tensor_tensor(out=pos, in0=pX[:, 64:96], in1=cnt[:], op=ALU.add), r=[B_pX, B_cnt], w=[B_rt])
        V(lambda: nc.vector.tensor_tensor(out=cnt[:], in0=cnt[:], in1=pX[:, 96:128], op=ALU.add), r=[B_pX, B_cnt], w=[B_cnt])
        for kk, ohk in enumerate((oh1, oh2)):
            V(lambda ohk=ohk: nc.vector.tensor_tensor(out=tmp32, in0=pos, in1=ohk, op=ALU.mult), r=[B_rt], w=[B_rt])
            V(lambda kk=kk, j=j: nc.vector.tensor_reduce(out=pe_all[:, j, kk:kk + 1], in_=tmp32, axis=AX.X, op=ALU.add), r=[B_rt], w=[B_dest[j]])
            V(lambda ohk=ohk: nc.vector.tensor_tensor(out=tmp32, in0=iota32, in1=ohk, op=ALU.mult), r=[B_rt, B_misc], w=[B_rt])
            V(lambda kk=kk, j=j: nc.vector.tensor_reduce(out=pe_all[:, j, 2 + kk:3 + kk], in_=tmp32, axis=AX.X, op=ALU.add), r=[B_rt], w=[B_dest[j]])
        yield

    mlstm_loads(NCH - 1, 0, scan_only=(NCTX > 0))
    pending = None
    for n_, j in enumerate(range(NCH - 1, -1, -1)):
        issue_casts(casts_per_iter)
        if NCTX > 0 and j == NOWN - 1:
            reset_state(False)
        if j - 1 >= 0:
            mlstm_loads(j - 1, (n_ + 1) % 2, scan_only=(j - 1 >= NOWN))
        if j >= NOWN:
            mlstm_chunk(j, True, n_ % 2, scan_only=True)
            continue
        Dsp(lambda j=j: nc.sync.dma_start(out=hfin[:], in_=HFs[j * 128:(j + 1) * 128, :]), r=[B_HF[j]], w=[B_hfin])
        rden = mlstm_chunk(j, True, n_ % 2, filler=pending)
        for h in range(8):
            V(lambda h=h, rden=rden: nc.vector.scalar_tensor_tensor(out=hout[:, h * 256:(h + 1) * 256], in0=hraw[:, h, :],
                                                                    scalar=rden[:, h:h + 1], in1=hfin[:, h * 256:(h + 1) * 256],
                                                                    op0=ALU.mult, op1=ALU.add), r=[B_hraw, B_cl, B_hfin], w=[B_hout])
        pending = merge_gen(j)
    if pending is not None:
        for _ in pending:
            pass
    issue_casts(len(cast_jobs))
    nblk = rt[:, 0:32]
    incl = rt[:, 32:64]
    t32 = rt[:, 64:96]
    bacc = rt[:, 96:224]
    t128 = rt[:, 224:352]
    V(lambda: nc.vector.memset(nblk, 0.0), w=[B_rt])
    for m_ in range(NTO * 2 // BLK + 1):
        V(lambda m_=m_: nc.vector.tensor_scalar(out=t32, in0=cnt[:], scalar1=float(m_ * BLK + 1), scalar2=None, op0=ALU.is_ge), r=[B_cnt], w=[B_rt])
        V(lambda: nc.vector.tensor_tensor(out=nblk, in0=nblk, in1=t32, op=ALU.add), r=[B_rt], w=[B_rt])
    V(lambda: nc.vector.tensor_tensor_scan(out=incl, data0=ones32, data1=nblk, initial=0.0, op0=ALU.mult, op1=ALU.add), r=[B_rt, B_misc], w=[B_rt])
    V(lambda: nc.vector.tensor_tensor(out=t32, in0=incl, in1=nblk, op=ALU.subtract), r=[B_rt], w=[B_rt])
    V(lambda: nc.vector.tensor_scalar(out=sslot[:], in0=t32, scalar1=float(BLK), scalar2=None, op0=ALU.mult), r=[B_rt], w=[B_ss])
    V(lambda: nc.vector.memset(bacc, 0.0), w=[B_rt])
    for e_ in range(NEXP):
        V(lambda e_=e_: nc.vector.tensor_scalar(out=t128, in0=iotaB, scalar1=incl[:, e_:e_ + 1], scalar2=None, op0=ALU.is_ge), r=[B_rt, B_misc], w=[B_rt])
        V(lambda: nc.vector.tensor_tensor(out=bacc, in0=bacc, in1=t128, op=ALU.add), r=[B_rt], w=[B_rt])
    V(lambda: nc.vector.tensor_scalar(out=bacc, in0=bacc, scalar1=float(NEXP - 1), scalar2=None, op0=ALU.min), r=[B_rt], w=[B_rt])
    for f_ in range(4):
        V(lambda f_=f_: nc.vector.tensor_scalar(out=t128, in0=bacc, scalar1=512.0, scalar2=float(f_ * 128), op0=ALU.mult, op1=ALU.add), r=[B_rt], w=[B_rt])
        V(lambda: nc.vector.tensor_scalar(out=t128, in0=t128, scalar1=iotap, scalar2=None, op0=ALU.add), r=[B_rt, B_misc], w=[B_rt])
        V(lambda f_=f_: nc.vector.tensor_copy(out=widx[:, f_, :], in_=t128), r=[B_rt], w=[B_bexp])
    dst = rt[:, 288:290]
    dst6 = rt[:, 288:294]
    isA = rt[:, 340:342]
    isB = rt[:, 342:344]
    ohx = rt[:, 0:32]
    for j in range(NOWN):
        for kk in range(2):
            V(lambda j=j, kk=kk: nc.vector.tensor_scalar(out=ohx, in0=iota32, scalar1=pe_all[:, j, 2 + kk:3 + kk], scalar2=None, op0=ALU.is_equal),
              r=[B_misc, B_dest[j]], w=[B_rt])
            V(lambda: nc.vector.tensor_tensor(out=ohx, in0=ohx, in1=sslot[:], op=ALU.mult), r=[B_rt, B_ss], w=[B_rt])
            V(lambda kk=kk: nc.vector.tensor_reduce(out=dst[:, kk:kk + 1], in_=ohx, axis=AX.X, op=ALU.add), r=[B_rt], w=[B_rt])
        V(lambda j=j: nc.vector.tensor_tensor(out=dst, in0=dst, in1=pe_all[:, j, 0:2], op=ALU.add), r=[B_rt, B_dest[j]], w=[B_rt])
        V(lambda j=j: nc.vector.tensor_scalar(out=dst, in0=dst, scalar1=float(-TRASH), scalar2=validt[:, j:j + 1], op0=ALU.add, op1=ALU.mult),
          r=[B_rt, B_misc], w=[B_rt])
        V(lambda: nc.vector.tensor_scalar(out=dst, in0=dst, scalar1=float(TRASH), scalar2=None, op0=ALU.add), r=[B_rt], w=[B_rt])
        V(lambda: nc.vector.tensor_scalar(out=isA, in0=dst, scalar1=float(NLh), scalar2=None, op0=ALU.is_lt), r=[B_rt], w=[B_rt])
        V(lambda: nc.vector.tensor_scalar(out=isB, in0=dst, scalar1=float(NL), scalar2=None, op0=ALU.is_lt), r=[B_rt], w=[B_rt])
        V(lambda: nc.vector.tensor_tensor(out=isB, in0=isB, in1=isA, op=ALU.subtract), r=[B_rt], w=[B_rt])
        V(lambda: nc.vector.scalar_tensor_tensor(out=rt[:, 290:292], in0=dst, scalar=float(-NLh), in1=isA, op0=ALU.add, op1=ALU.mult), r=[B_rt], w=[B_rt])
        V(lambda: nc.vector.scalar_tensor_tensor(out=rt[:, 292:294], in0=dst, scalar=float(-NL), in1=isB, op0=ALU.add, op1=ALU.mult), r=[B_rt], w=[B_rt])
        V(lambda: nc.vector.tensor_scalar(out=rt[:, 290:294], in0=rt[:, 290:294], scalar1=float(NLh), scalar2=None, op0=ALU.add), r=[B_rt], w=[B_rt])
        V(lambda j=j: nc.vector.tensor_copy(out=dest_all[:, j, :], in_=dst6), r=[B_rt], w=[B_dest[j]])
        V(lambda j=j: nc.vector.tensor_scalar(out=rt[:, 296:297], in0=iotap, scalar1=float(j * 128), scalar2=None, op0=ALU.add),
          r=[B_misc], w=[B_rt])
        V(lambda: nc.vector.tensor_copy(out=tokid[:], in_=rt[:, 296:297]), r=[B_rt], w=[B_tokid])
        for kk in range(2):
            Dpl(lambda j=j, kk=kk: nc.gpsimd.indirect_dma_start(out=Ls[:, :], out_offset=bass.IndirectOffsetOnAxis(ap=dest_all[:, j, kk:kk + 1], axis=0),
                                                               in_=tokid[:, :], in_offset=None),
                r=[B_dest[j], B_tokid, B_L], w=[B_L])
    S.flush()
    P.release(mk)

    mk = P.mark()
    NSB = BLK // 128
    idxe = [P.sb("idxe%d" % i, [128, NSB], I32) for i in range(2)]
    B_idxe = [Buf("idxe%d" % i) for i in range(2)]
    xg = [P.sb("xg%d" % i, [128, D], BF16) for i in range(3)]
    B_xg = [Buf("xg%d" % i) for i in range(3)]
    xeT = [P.sb("xeT%d" % i, [128, 16, BLK], BF16) for i in range(2)]
    B_xeT = [Buf("xeT%d" % i) for i in range(2)]
    hT = P.sb("hT", [128, 8, BLK], BF16)
    B_hT = Buf("hT")
    wgs = [P.sb("wgs%d" % i, [128, 16, 256], BF16) for i in range(3)]
    wus = [P.sb("wus%d" % i, [128, 16, 256], BF16) for i in range(3)]
    B_wgs = [Buf("wgs%d" % i) for i in range(3)]
    B_wus = [Buf("wus%d" % i) for i in range(3)]
    wds = [P.sb("wds%d" % i, [128, 8, 512], BF16) for i in range(3)]
    B_wds = [Buf("wds%d" % i) for i in range(3)]
    sg = [P.sb("sg%d" % i, [128, 512], F32) for i in range(2)]
    B_sg = [Buf("sg%d" % i) for i in range(2)]
    yb = [P.sb("yb%d" % i, [128, 512], F32) for i in range(3)]
    B_yb = [Buf("yb%d" % i) for i in range(3)]
    ptr = P.ps("ptr4", [128, 2048], BF16)
    B_ptr = Buf("ptr4")
    pG = [P.ps("pG%d" % i, [128, 512], F32) for i in range(2)]
    pU = [P.ps("pU%d" % i, [128, 512], F32) for i in range(2)]
    B_pG = [Buf("pG%d" % i) for i in range(2)]
    B_pU = [Buf("pU%d" % i) for i in range(2)]
    pY = [P.ps("pY%d" % i, [128, 512], F32) for i in range(2)]
    B_pY = [Buf("pY%d" % i) for i in range(2)]
    cq = {"w": 0, "d": 0, "g": 0, "y": 0, "p": 0, "s": 0, "st": 0}
    def dyn_load(dst_tile, src2, b, f_, Bd):
        Dpl(lambda: nc.gpsimd.indirect_dma_start(out=dst_tile[:].rearrange("p k n -> p (k n)"), out_offset=None, in_=src2[:, :],
                                                 in_offset=bass.IndirectOffsetOnAxis(ap=widx[:, f_, b:b + 1], axis=0)),
            r=[B_bexp, B_wexp], w=[Bd])

    for b in range(NBLK):
        ie = b % 2
        xT = xeT[ie]
        BxT = B_xeT[ie]
        Dsp(lambda b=b, ie=ie: nc.sync.dma_start(out=idxe[ie][:], in_=Ls[b * BLK:(b + 1) * BLK, :].rearrange("(s p) o -> p (s o)", p=128),
                                                 allow_slow_non_contiguous=True),
            r=[B_L], w=[B_idxe[ie]])
        for sb_ in range(NSB):
            gi_ = cq["g"] % 3
            cq["g"] += 1
            Dpl(lambda ie=ie, sb_=sb_, gi_=gi_: nc.gpsimd.indirect_dma_start(out=xg[gi_][:, :], out_offset=None, in_=H2s[:, :],
                                                                             in_offset=bass.IndirectOffsetOnAxis(ap=idxe[ie][:, sb_:sb_ + 1], axis=0)),
                r=[B_idxe[ie], B_H2], w=[B_xg[gi_]])
            for jj in range(16):
                T(lambda jj=jj, gi_=gi_: nc.tensor.transpose(out=ptr[:, jj * 128:(jj + 1) * 128], in_=xg[gi_][:, jj * 128:(jj + 1) * 128],
                                                             identity=identb[:]), r=[B_xg[gi_], B_cb], w=[B_ptr])
            A(lambda sb_=sb_, xT=xT: nc.scalar.copy(out=xT[:, 0:8, sb_ * 128:(sb_ + 1) * 128], in_=ptr[:, 0:1024].rearrange("p (k t) -> p k t", k=8)),
              r=[B_ptr], w=[BxT])
            V(lambda sb_=sb_, xT=xT: nc.vector.tensor_copy(out=xT[:, 8:16, sb_ * 128:(sb_ + 1) * 128], in_=ptr[:, 1024:2048].rearrange("p (k t) -> p k t", k=8)),
              r=[B_ptr], w=[BxT])
        for fs in range(4):
            wi = cq["w"] % 3
            cq["w"] += 1
            dyn_load(wgs[wi], WGB, b, fs, B_wgs[wi])
            dyn_load(wus[wi], WUB, b, fs, B_wus[wi])
            for fc in range(2):
                f8 = fs * 2 + fc
                pi = cq["p"] % 2
                cq["p"] += 1
                for k in range(16):
                    T(lambda k=k, fc=fc, wi=wi, pi=pi, xT=xT: nc.tensor.matmul(pG[pi][:, :], lhsT=wgs[wi][:, k, fc * 128:(fc + 1) * 128],
                                                                              rhs=xT[:, k, :], start=(k == 0), stop=(k == 15)),
                      r=[B_wgs[wi], BxT], w=[B_pG[pi]])
                for k in range(16):
                    T(lambda k=k, fc=fc, wi=wi, pi=pi, xT=xT: nc.tensor.matmul(pU[pi][:, :], lhsT=wus[wi][:, k, fc * 128:(fc + 1) * 128],
                                                                              rhs=xT[:, k, :], start=(k == 0), stop=(k == 15)),
                      r=[B_wus[wi], BxT], w=[B_pU[pi]])
                si = cq["s"] % 2
                cq["s"] += 1
                A(lambda pi=pi, si=si: nc.scalar.activation(out=sg[si][:], in_=pG[pi][:, :], func=AF.Silu), r=[B_pG[pi]], w=[B_sg[si]])
                V(lambda pi=pi, si=si, f8=f8: nc.vector.tensor_tensor(out=hT[:, f8, :], in0=sg[si][:], in1=pU[pi][:, :], op=ALU.mult),
                  r=[B_sg[si], B_pU[pi]], w=[B_hT])
        for ns in range(4):
            di = cq["d"] % 3
            cq["d"] += 1
            dyn_load(wds[di], WDB, b, ns, B_wds[di])
            for sb_ in range(NSB):
                pi = cq["y"] % 2
                yi = cq["y"] % 3
                cq["y"] += 1
                for kf in range(8):
                    T(lambda kf=kf, sb_=sb_, di=di, pi=pi: nc.tensor.matmul(pY[pi][:, :], lhsT=hT[:, kf, sb_ * 128:(sb_ + 1) * 128], rhs=wds[di][:, kf, :],
                                                                          start=(kf == 0), stop=(kf == 7)), r=[B_hT, B_wds[di]], w=[B_pY[pi]])
                if yi % 2 == 0:
                    A(lambda pi=pi, yi=yi: nc.scalar.copy(out=yb[yi][:], in_=pY[pi][:, :]), r=[B_pY[pi]], w=[B_yb[yi]])
                else:
                    V(lambda pi=pi, yi=yi: nc.vector.tensor_copy(out=yb[yi][:], in_=pY[pi][:, :]), r=[B_pY[pi]], w=[B_yb[yi]])
                Yh = YsA if b < NBLK // 2 else YsB
                eo = (b % (NBLK // 2)) * BLK + sb_ * 128
                S.dma("act", lambda eo=eo, Yh=Yh, ns=ns, yi=yi: nc.scalar.dma_start(out=Yh[eo:eo + 128, ns * 512:(ns + 1) * 512], in_=yb[yi][:]),
                      [B_yb[yi]], [B_Y])
    S.flush()
    P.release(mk)

    mk = P.mark()
    x5 = [P.sb("x5%d" % i, [128, D], F32) for i in range(2)]
    y0 = [P.sb("y0%d" % i, [128, D], F32) for i in range(2)]
    y1 = [P.sb("y1%d" % i, [128, D], F32) for i in range(2)]
    y2 = [P.sb("y2%d" % i, [128, D], F32) for i in range(2)]
    y3 = [P.sb("y3%d" % i, [128, D], F32) for i in range(2)]
    B_y2 = [Buf("y2%d" % i) for i in range(2)]
    B_y3 = [Buf("y3%d" % i) for i in range(2)]
    B_x5 = [Buf("x5%d" % i) for i in range(2)]
    B_y0 = [Buf("y0%d" % i) for i in range(2)]
    B_y1 = [Buf("y1%d" % i) for i in range(2)]
    gfb = P.sb("gfb", [128, D], F32)
    B_gf = Buf("gfb")
    junk = P.sb("junk5", [128, D], BF16)
    B_junk = Buf("junk5")
    sm = P.sb("sm5", [128, 8], F32)
    B_sm = Buf("sm5")
    Dsp(lambda: nc.sync.dma_start(out=gfb[:], in_=bcast_row(g_fin_d, D)), w=[B_gf])
    for j in range(NOWN):
        i = j % 2
        Dsp(lambda j=j, i=i: nc.sync.dma_start(out=x5[i][:], in_=X1s[j * 128:(j + 1) * 128, :]), r=[B_X1[j]], w=[B_x5[i]])
        for (yt_, By_, Yh, col) in ((y0, B_y0, YsA, 2), (y1, B_y1, YsA, 3), (y2, B_y2, YsB, 4), (y3, B_y3, YsB, 5)):
            Dpl(lambda j=j, i=i, yt_=yt_, Yh=Yh, col=col: nc.gpsimd.indirect_dma_start(
                out=yt_[i][:, :], out_offset=None, in_=Yh[:, :],
                in_offset=bass.IndirectOffsetOnAxis(ap=dest_all[:, j, col:col + 1], axis=0)),
                r=[B_dest[j], B_Y], w=[By_[i]])
        V(lambda i=i: nc.vector.tensor_tensor(out=y0[i][:], in0=y0[i][:], in1=y2[i][:], op=ALU.add), r=[B_y0[i], B_y2[i]], w=[B_y0[i]])
        V(lambda i=i: nc.vector.tensor_tensor(out=y1[i][:], in0=y1[i][:], in1=y3[i][:], op=ALU.add), r=[B_y1[i], B_y3[i]], w=[B_y1[i]])
        V(lambda j=j, i=i: nc.vector.scalar_tensor_tensor(out=x5[i][:], in0=y0[i][:], scalar=w_all[:, j, 0:1], in1=x5[i][:],
                                                          op0=ALU.mult, op1=ALU.add), r=[B_y0[i], B_dest[j], B_x5[i]], w=[B_x5[i]])
        V(lambda j=j, i=i: nc.vector.scalar_tensor_tensor(out=x5[i][:], in0=y1[i][:], scalar=w_all[:, j, 1:2], in1=x5[i][:],
                                                          op0=ALU.mult, op1=ALU.add), r=[B_y1[i], B_dest[j], B_x5[i]], w=[B_x5[i]])
        G(lambda: nc.gpsimd.memset(sm[:, 0:1], 0.0), w=[B_sm])
        A(lambda i=i: nc.scalar.activation(out=junk[:], in_=x5[i][:], func=AF.Square, accum_out=sm[:, 0:1]), r=[B_x5[i], B_sm], w=[B_junk, B_sm])
        A(lambda: nc.scalar.activation(out=sm[:, 1:2], in_=sm[:, 0:1], func=AF.Sqrt, bias=epsb[:, 0:1], scale=1.0 / D), r=[B_sm, B_misc], w=[B_sm])
        V(lambda: nc.vector.reciprocal(out=sm[:, 1:2], in_=sm[:, 1:2]), r=[B_sm], w=[B_sm])
        V(lambda i=i: nc.vector.scalar_tensor_tensor(out=y0[i][:], in0=x5[i][:], scalar=sm[:, 1:2], in1=gfb[:], op0=ALU.mult, op1=ALU.mult),
          r=[B_x5[i], B_sm, B_gf], w=[B_y0[i]])
        Dsp(lambda j=j, i=i: nc.sync.dma_start(out=y_d[j * 128:(j + 1) * 128, :], in_=y0[i][:]), r=[B_y0[i]])
    S.flush()
    P.release(mk)
    return nc, S


def host_consts():
    c = np.zeros((128, 1024), np.float32)
    i = np.arange(128)
    c[:, 0:128] = np.eye(128)
    s, t = np.meshgrid(i, i, indexing="ij")
    c[:, 128:256] = (s <= t)
    c[:, 256:384] = np.where(t <= s, 0.0, NEG)
    c[:, 384:512] = np.where(s <= t, 0.0, NEG)
    c[:, 512:640] = (s >= t)
    c[:, 640:768] = np.where(t >= s, 0.0, NEG)
    c[:, 768:896] = np.where(s >= t, 0.0, NEG)
    c[:, 896:1024] = (s < t)
    sel = np.zeros((8, 8 * 128 + 128 + 8), np.float32)
    for h in range(8):
        sel[h, h * 128:(h + 1) * 128] = 1.0
    sel[:, 1024:1152] = 1.0
    sel[:, 1152:1160] = np.eye(8)
    return c, sel


def host_rcst(CAP, NTO):
    r = np.zeros((128, 240), np.float32)
    r[:, 0:8] = 1.0
    r[:, 40] = np.arange(128)
    r[:, 41] = 1.0
    r[:, 48:80] = np.arange(32)[None, :]
    r[:, 80:208] = np.arange(128)[None, :]
    r[:, 208:240] = 1.0
    nblk = (NTO * 2) // 512 + NEXP
    nblk += nblk % 2
    NL = nblk * 512
    padi = np.full((128, NL // 128 + 1), NTO, np.int32)
    return r, padi


def shared_inputs(inp):
    f = lambda a: np.ascontiguousarray(np.asarray(a, dtype=np.float32))
    cst, sel = host_consts()
    conv = f(inp["mlstm_conv_w"])[0]
    return {
        "w_in": f(inp["w_in"])[0],
        "g_mix": f(inp["norm_mix_g"]).reshape(1, D),
        "g_ffn": f(inp["norm_ffn_g"]).reshape(1, D),
        "g_fin": f(inp["norm_final_g"]).reshape(1, D),
        "b_cg": f(inp["b_cell_gates"]).reshape(1, 32),
        "ln_g": f(inp["gmlp_ln_g"]).reshape(1, D),
        "wsT": np.ascontiguousarray(f(inp["gmlp_w_s"])[0].transpose(2, 0, 1)),
        "bsT": np.ascontiguousarray(f(inp["gmlp_b_s"])[0].T),
        "convw": np.ascontiguousarray(conv.reshape(5, 32, 128).transpose(2, 1, 0)),
        "headg": f(inp["mlstm_head_g"]).reshape(1, D),
        "w_out": f(inp["w_out"])[0],
        "w_r": np.ascontiguousarray(np.concatenate([f(inp["w_router_group"])[0], f(inp["w_router_expert"])[0]], axis=1)),
        "b_r": np.concatenate([f(inp["b_router_group"]).reshape(1, 4), f(inp["b_router_expert"]).reshape(1, 32)], axis=1),
        "w_gate": np.ascontiguousarray(f(inp["w_exp_gate"])[0].reshape(NEXP, 16, 128, 4, 256).transpose(0, 3, 2, 1, 4)).reshape(NEXP * 4 * 128, 16 * 256),
        "w_up": np.ascontiguousarray(f(inp["w_exp_up"])[0].reshape(NEXP, 16, 128, 4, 256).transpose(0, 3, 2, 1, 4)).reshape(NEXP * 4 * 128, 16 * 256),
        "w_down": np.ascontiguousarray(f(inp["w_exp_down"])[0].reshape(NEXP, 8, 128, 4, 512).transpose(0, 3, 2, 1, 4)).reshape(NEXP * 4 * 128, 8 * 512),
        "cst": cst,
        "sel": sel,
    }


NOWN_FULL = 64
NCTX_FULL = 64
CAP_FULL = 768


def flip_shared(sh):
    f = dict(sh)
    w = sh["w_in"].copy()
    w[:, 12288:12304] = sh["w_in"][:, 12304:12320]
    w[:, 12304:12320] = sh["w_in"][:, 12288:12304]
    f["w_in"] = w
    b = sh["b_cg"].copy()
    b[:, 0:16] = sh["b_cg"][:, 16:32]
    b[:, 16:32] = sh["b_cg"][:, 0:16]
    f["b_cg"] = b
    f["wsT"] = np.ascontiguousarray(sh["wsT"][::-1, :, ::-1])
    f["bsT"] = np.ascontiguousarray(sh["bsT"][::-1])
    f["convw"] = np.ascontiguousarray(sh["convw"][:, :, ::-1])
    return f


def kernel(**inp):
    xp = np.asarray(inp["x_prompt"], dtype=np.float32)
    xs = np.asarray(inp["x_sample"], dtype=np.float32)
    NTO = NOWN_FULL * 128
    NT = (NOWN_FULL + NCTX_FULL) * 128
    sh = shared_inputs(inp)
    sh["rcst"], sh["padi"] = host_rcst(CAP_FULL, NTO)
    shf = flip_shared(sh)
    nc, S = build(NOWN_FULL, NCTX_FULL, CAP_FULL)
    in_maps = []
    ones_v = np.ones((128, NOWN_FULL), np.float32)
    for c in range(8):
        m = dict(shf if c == 5 else sh)
        x = np.zeros((NT, D), np.float32)
        valid = np.zeros((128, NOWN_FULL), np.float32)
        keep = np.zeros((128, 1), np.float32)
        if c < 4:
            x[:NTO] = xp[c]
            valid = ones_v
        elif c == 4:
            x[:] = xs[0]
            valid = ones_v
            keep[:] = 1.0
        elif c == 5:
            x[:] = xs[0][::-1]
            valid = ones_v
            keep[:] = 1.0
        m.update({"x": x, "valid": valid, "keep": keep})
        in_maps.append(m)
    res = run_bass_kernel_spmd(nc, in_maps, core_ids=list(range(8)))
    yp = np.stack([res.results[c]["y"] for c in range(4)], axis=0).astype(np.float32)
    ys = np.concatenate([res.results[4]["y"], res.results[5]["y"][::-1]], axis=0)[None].astype(np.float32)
    return (yp, ys)
```
